# Optimizing a Trainium2 kernel written in Bass

```python
import jax, jax.numpy as jnp
from jax import lax
import numpy as np

D_MODEL = 2048
BATCH = 2
SEQ = 4096
DEPTH = 4

D_MIX = D_MODEL
ATTN_HEADS = 16
ATTN_KV_HEADS = 4
ATTN_HEAD_DIM = 64
IDX_HEADS = 8
IDX_DIM = 64
IDX_TOPK_MAX = 256
Q_BLOCK = 128
CONV_DIM = 512
CONV_WIDTH = 3
RET_HEADS = 4
RET_HEAD_DIM = 128
RET_CHUNK = 128
ROPE_BASE = 10000.0
N_EXPERTS = 32
TOP_K = 4
D_FF = 768
SWIGLU_LIMIT = 7.0
SWIGLU_ALPHA = 1.702
LN_EPS = 1e-5
DEEPNORM_ALPHA = (2 * DEPTH) ** 0.25
DEEPNORM_BETA = (8 * DEPTH) ** -0.25

ATTN_W = ATTN_HEADS * ATTN_HEAD_DIM
KV_W = ATTN_KV_HEADS * ATTN_HEAD_DIM
RET_W = RET_HEADS * RET_HEAD_DIM
IN_SIZES = (ATTN_W, KV_W, KV_W, IDX_HEADS * IDX_DIM, IDX_DIM, IDX_HEADS,
            CONV_DIM, CONV_DIM, CONV_DIM, RET_W, RET_W, RET_W, RET_W)
VALUE_SLOTS = (2, 8, 11)
N_IN = sum(IN_SIZES)

kernel_name = 'hybrid_dsa_conv_retention_moe_deepnorm'


def _split_points():
    pts, acc = [], 0
    for s in IN_SIZES[:-1]:
        acc += s
        pts.append(acc)
    return pts


def layer_norm(x, g, b):
    xf = x.astype(jnp.float32)
    mu = jnp.mean(xf, axis=-1, keepdims=True)
    var = jnp.mean(jnp.square(xf - mu), axis=-1, keepdims=True)
    y = (xf - mu) * lax.rsqrt(var + LN_EPS) * g.astype(jnp.float32) + b.astype(jnp.float32)
    return y.astype(x.dtype)


def dsa_attention(q, k, v, qi, ki, wi, topk):
    B, L = q.shape[:2]
    nb = L // Q_BLOCK
    rep = ATTN_HEADS // ATTN_KV_HEADS
    scale = ATTN_HEAD_DIM ** -0.5
    key_pos = jnp.arange(L)

    def to_blocks(a):
        return a.reshape(B, nb, Q_BLOCK, *a.shape[2:]).swapaxes(0, 1)

    def one_block(args):
        qb, qib, wib, start = args
        qpos = start + jnp.arange(Q_BLOCK)
        rel = jax.nn.relu(jnp.einsum('bqhd,bsd->bqhs', qib, ki))
        score = jnp.einsum('bqhs,bqh->bqs', rel, wib).astype(jnp.float32)
        causal = key_pos[None, :] <= qpos[:, None]
        score = jnp.where(causal[None], score, -jnp.inf)
        _, idx = lax.top_k(score, topk)
        valid = idx <= qpos[None, :, None]
        kg = jax.vmap(lambda kk, ii: kk[ii])(k, idx)
        vg = jax.vmap(lambda vv, ii: vv[ii])(v, idx)
        qg = qb.reshape(B, Q_BLOCK, ATTN_KV_HEADS, rep, ATTN_HEAD_DIM)
        s = jnp.einsum('bqgrd,bqkgd->bqgrk', qg, kg).astype(jnp.float32) * scale
        s = jnp.where(valid[:, :, None, None, :], s, -jnp.inf)
        p = jax.nn.softmax(s, axis=-1).astype(v.dtype)
        o = jnp.einsum('bqgrk,bqkgd->bqgrd', p, vg)
        return o.reshape(B, Q_BLOCK, ATTN_HEADS * ATTN_HEAD_DIM)

    starts = jnp.arange(nb, dtype=jnp.int32) * Q_BLOCK
    out = lax.map(one_block, (to_blocks(q), to_blocks(qi), to_blocks(wi), starts))
    return out.swapaxes(0, 1).reshape(B, L, ATTN_HEADS * ATTN_HEAD_DIM)


def short_conv(b_gate, c_gate, h, conv_w):
    u = c_gate * h
    y = lax.conv_general_dilated(u, conv_w[:, None, :], window_strides=(1,),
                                 padding=[(CONV_WIDTH - 1, 0)],
                                 dimension_numbers=('NWC', 'WIO', 'NWC'),
                                 feature_group_count=CONV_DIM)
    return b_gate * y


def rotary(x, pos):
    half = x.shape[-1] // 2
    inv_freq = ROPE_BASE ** (-jnp.linspace(0.0, 1.0, half, dtype=jnp.float32))
    ang = pos.astype(jnp.float32)[:, None] * inv_freq[None, :]
    cos = jnp.cos(ang)[None, :, None, :]
    sin = jnp.sin(ang)[None, :, None, :]
    xf = x.astype(jnp.float32)
    x1, x2 = xf[..., :half], xf[..., half:]
    return jnp.concatenate([x1 * cos - x2 * sin, x1 * sin + x2 * cos], axis=-1).astype(x.dtype)


def retention(q, k, v):
    B, L, H, dk = q.shape
    dv = v.shape[-1]
    dt = q.dtype
    nc = L // RET_CHUNK
    lg = jnp.log1p(-jnp.exp2(-5.0 - jnp.arange(H, dtype=jnp.float32)))
    i = jnp.arange(RET_CHUNK, dtype=jnp.float32)
    diff = i[:, None] - i[None, :]
    intra = jnp.where(diff[None] >= 0,
                      jnp.exp(jnp.maximum(diff, 0.0)[None] * lg[:, None, None]), 0.0).astype(dt)
    q_dec = jnp.exp((i + 1.0)[None, :] * lg[:, None]).astype(dt)[..., None]
    k_dec = jnp.exp((RET_CHUNK - 1.0 - i)[None, :] * lg[:, None]).astype(dt)[..., None]
    c_dec = jnp.exp(RET_CHUNK * lg).astype(dt)[:, None, None]

    def chunks(a):
        return a.reshape(B, nc, RET_CHUNK, H, a.shape[-1]).transpose(1, 0, 3, 2, 4)

    def step(state, inp):
        qc, kc, vc = inp
        s = jnp.einsum('bhid,bhjd->bhij', qc, kc) * intra
        o = (jnp.einsum('bhij,bhje->bhie', s, vc)
             + jnp.einsum('bhid,bhde->bhie', qc * q_dec, state))
        state = state * c_dec + jnp.einsum('bhjd,bhje->bhde', kc * k_dec, vc)
        return state, o

    state0 = jnp.zeros((B, H, dk, dv), dt)
    _, o = lax.scan(step, state0, (chunks(q), chunks(k), chunks(v)))
    return o.transpose(1, 0, 3, 2, 4).reshape(B, L, H, dv)


def head_group_norm(o):
    of = o.astype(jnp.float32)
    mu = jnp.mean(of, axis=-1, keepdims=True)
    var = jnp.mean(jnp.square(of - mu), axis=-1, keepdims=True)
    return ((of - mu) * lax.rsqrt(var + LN_EPS)).astype(o.dtype)


def hybrid_mixer(x, w_in, conv_w, w_out):
    B, L, _ = x.shape
    proj = x @ w_in
    (aq, ak, av, iq, ik, iw, cb, cc, ch, rq, rk, rv, rg) = jnp.split(proj, _split_points(), axis=-1)
    topk = min(IDX_TOPK_MAX, L // 4)
    attn = dsa_attention(aq.reshape(B, L, ATTN_HEADS, ATTN_HEAD_DIM),
                         ak.reshape(B, L, ATTN_KV_HEADS, ATTN_HEAD_DIM),
                         av.reshape(B, L, ATTN_KV_HEADS, ATTN_HEAD_DIM),
                         iq.reshape(B, L, IDX_HEADS, IDX_DIM), ik,
                         iw * (IDX_HEADS * IDX_DIM) ** -0.5, topk)
    conv = short_conv(cb, cc, ch, conv_w)
    pos = jnp.arange(L)
    rq = rotary(rq.reshape(B, L, RET_HEADS, RET_HEAD_DIM), pos)
    rk = rotary(rk.reshape(B, L, RET_HEADS, RET_HEAD_DIM), pos) * (RET_HEAD_DIM ** -0.5)
    ret = retention(rq, rk, rv.reshape(B, L, RET_HEADS, RET_HEAD_DIM))
    ret = head_group_norm(ret).reshape(B, L, RET_W) * jax.nn.silu(rg)
    return jnp.concatenate([attn, conv, ret], axis=-1) @ w_out


def expert_ffn(xt, w_gu, b_gu, w_down, b_down):
    h = xt @ w_gu + b_gu
    gate = jnp.minimum(h[..., ::2], SWIGLU_LIMIT)
    up = jnp.clip(h[..., 1::2], -SWIGLU_LIMIT, SWIGLU_LIMIT)
    glu = gate * jax.nn.sigmoid(SWIGLU_ALPHA * gate)
    return ((up + 1.0) * glu) @ w_down + b_down


def moe(x, router_w, router_b, w_gu, b_gu, w_down, b_down):
    B, L, D = x.shape
    xt = x.reshape(B * L, D)
    logits = (xt @ router_w + router_b).astype(jnp.float32)
    top_val, top_idx = lax.top_k(logits, TOP_K)
    gates = jax.nn.softmax(top_val, axis=-1)
    combine = jnp.einsum('tk,tke->te', gates,
                         jax.nn.one_hot(top_idx, N_EXPERTS, dtype=jnp.float32)).astype(x.dtype)
    out = jnp.zeros_like(xt)
    for e in range(N_EXPERTS):
        out = out + combine[:, e:e + 1] * expert_ffn(xt, w_gu[e], b_gu[e], w_down[e], b_down[e])
    return out.reshape(B, L, D)


def setup_inputs(seed: int = 0) -> dict:
    key = jax.random.key(seed)
    ks = jax.random.split(key, 16)
    f32 = jnp.float32
    x = jax.random.normal(ks[0], (BATCH, SEQ, D_MODEL), f32)
    col_scale = np.ones((N_IN,), np.float32)
    starts = [0] + _split_points()
    for slot in VALUE_SLOTS:
        col_scale[starts[slot]:starts[slot] + IN_SIZES[slot]] = DEEPNORM_BETA
    w_in = jax.random.normal(ks[1], (DEPTH, D_MODEL, N_IN), f32) * (D_MODEL ** -0.5) * jnp.asarray(col_scale)
    conv_w = jax.random.normal(ks[2], (DEPTH, CONV_WIDTH, CONV_DIM), f32) * (CONV_WIDTH ** -0.5)
    w_out = jax.random.normal(ks[3], (DEPTH, D_MIX, D_MODEL), f32) * (D_MIX ** -0.5 * DEEPNORM_BETA)
    ln1_g = 1.0 + 0.02 * jax.random.normal(ks[4], (DEPTH, D_MODEL), f32)
    ln1_b = 0.02 * jax.random.normal(ks[5], (DEPTH, D_MODEL), f32)
    router_w = jax.random.normal(ks[6], (DEPTH, D_MODEL, N_EXPERTS), f32) * (D_MODEL ** -0.5)
    router_b = 0.01 * jax.random.normal(ks[7], (DEPTH, N_EXPERTS), f32)
    w_gu = jax.random.normal(ks[8], (DEPTH, N_EXPERTS, D_MODEL, 2 * D_FF), f32) * (D_MODEL ** -0.5)
    b_gu = 0.01 * jax.random.normal(ks[9], (DEPTH, N_EXPERTS, 2 * D_FF), f32)
    w_down = jax.random.normal(ks[10], (DEPTH, N_EXPERTS, D_FF, D_MODEL), f32) * (D_FF ** -0.5 * DEEPNORM_BETA)
    b_down = 0.01 * jax.random.normal(ks[11], (DEPTH, N_EXPERTS, D_MODEL), f32)
    ln2_g = 1.0 + 0.02 * jax.random.normal(ks[12], (DEPTH, D_MODEL), f32)
    ln2_b = 0.02 * jax.random.normal(ks[13], (DEPTH, D_MODEL), f32)
    return {'x': x, 'w_in': w_in, 'conv_w': conv_w, 'w_out': w_out,
            'ln1_g': ln1_g, 'ln1_b': ln1_b, 'router_w': router_w, 'router_b': router_b,
            'w_gu': w_gu, 'b_gu': b_gu, 'w_down': w_down, 'b_down': b_down,
            'ln2_g': ln2_g, 'ln2_b': ln2_b}


def reference(x, w_in, conv_w, w_out, ln1_g, ln1_b, router_w, router_b,
              w_gu, b_gu, w_down, b_down, ln2_g, ln2_b):
    for l in range(DEPTH):
        mix = hybrid_mixer(x, w_in[l], conv_w[l], w_out[l])
        x = layer_norm(DEEPNORM_ALPHA * x + mix, ln1_g[l], ln1_b[l])
        ffn = moe(x, router_w[l], router_b[l], w_gu[l], b_gu[l], w_down[l], b_down[l])
        x = layer_norm(DEEPNORM_ALPHA * x + ffn, ln2_g[l], ln2_b[l])
    return x
```

```python
import numpy as np
from contextlib import ExitStack
import concourse.bass as bass
import concourse.mybir as mybir
from concourse.bass_utils import run_bass_kernel_spmd

F32 = mybir.dt.float32
BF16 = mybir.dt.bfloat16
ALU = mybir.AluOpType
AF = mybir.ActivationFunctionType
AX = mybir.AxisListType

D = 2048
L = 4096
NT = 32
DEPTH = 4
NE = 32
DFF = 768
ALPHA = (2 * DEPTH) ** 0.25
EPS = 1e-5
TOPK = 256
NBIS = 18
GT = 4
NG = NT // GT
GW = GT * 128
NFC = 27
FCOLS = NFC * 128
TCOLS = 2312
NCOL = FCOLS + TCOLS
EPOCH = 30000


class Buf:
    __slots__ = ("ap", "w", "r", "name")

    def __init__(self, ap, name=""):
        self.ap = ap
        self.w = None
        self.r = {}
        self.name = name

    def __getitem__(self, idx):
        return self.ap[idx]


class Sched:
    ENG = ("pe", "act", "dve", "pool", "sp")

    def __init__(self, nc, es, n_dma_sems=32):
        self.nc = nc
        self.es = es
        self.e = {"pe": nc.tensor, "act": nc.scalar, "dve": nc.vector, "pool": nc.gpsimd, "sp": nc.sync}
        self.sem = {}
        self.cnt = {}
        self.tot = {k: 0 for k in self.ENG}
        self.nsem = 0
        for k in self.ENG:
            self.sem[k] = self._newsem(k)
            self.cnt[k] = 0
        self.dma_sems = [[self._newsem("dma"), 0] for _ in range(n_dma_sems)]
        self.dma_rr = 0
        self.seen = {k: {} for k in self.ENG}

    def _newsem(self, name):
        self.nsem += 1
        return self.es.enter_context(self.nc.semaphore(f"{name}_{self.nsem}"))

    def _wait(self, eng, ev):
        if ev is None:
            return
        sem, val, src = ev
        if src == "pe" and eng == "pe":
            return
        s = self.seen[eng]
        if s.get(id(sem), 0) >= val:
            return
        self.e[eng].wait_ge(sem, val)
        s[id(sem)] = val

    def _deps(self, eng, reads, writes):
        for b in reads:
            self._wait(eng, b.w)
        for b in writes:
            self._wait(eng, b.w)
            for ev in b.r.values():
                self._wait(eng, ev)

    def _mark(self, ev, reads, writes, key):
        for b in reads:
            b.r[key] = ev
        for b in writes:
            b.w = ev
            b.r = {}

    def op(self, eng, fn, reads=(), writes=()):
        self._deps(eng, reads, writes)
        inst = fn(self.e[eng])
        if self.cnt[eng] >= EPOCH:
            self.sem[eng] = self._newsem(eng)
            self.cnt[eng] = 0
        self.cnt[eng] += 1
        self.tot[eng] += 1
        sem = self.sem[eng]
        inst.then_inc(sem, 1)
        ev = (sem, self.cnt[eng], eng)
        self._mark(ev, reads, writes, eng)
        return ev

    def dma(self, eng, out, in_, reads=(), writes=(), **kw):
        self._deps(eng, reads, writes)
        slot = self.dma_sems[self.dma_rr]
        self.dma_rr = (self.dma_rr + 1) % len(self.dma_sems)
        sem = slot[0]
        if slot[1] > 0:
            self._wait(eng, (sem, slot[1], "dma"))
        inst = self.e[eng].dma_start(out=out, in_=in_, **kw)
        slot[1] += 16
        inst.then_inc(sem, 16)
        ev = (sem, slot[1], "dma")
        self.tot[eng] += 1
        self._mark(ev, reads, writes, "dma%d" % id(sem))
        return ev

    def barrier(self, bufs):
        for eng in self.ENG:
            for b in bufs:
                self._wait(eng, b.w)
                for ev in b.r.values():
                    self._wait(eng, ev)

    def drain_dmas(self, eng="sp"):
        for sem, val in self.dma_sems:
            if val > 0:
                self._wait(eng, (sem, val, "dma"))


def build_program(n_layers=DEPTH, do_moe=True, dbg=False):
    nc = bass.Bass("TRN2", target_bir_lowering=False)
    P = 128

    def din(name, shape):
        return nc.dram_tensor(name, list(shape), F32, kind="ExternalInput").ap()

    x_in = din("x", [L, D])
    w_in = din("w_in", [n_layers, D, NCOL])
    conv_w = din("conv_w", [n_layers, 3, 512])
    w_out = din("w_out", [n_layers, D, D])
    ln1_g = din("ln1_g", [n_layers, D]); ln1_b = din("ln1_b", [n_layers, D])
    ln2_g = din("ln2_g", [n_layers, D]); ln2_b = din("ln2_b", [n_layers, D])
    if do_moe:
        router_w = din("router_w", [n_layers, D, NE]); router_b = din("router_b", [n_layers, NE])
        w_gu = din("w_gu", [n_layers, NE, D, 2 * DFF]); b_gu = din("b_gu", [n_layers, NE, 2 * DFF])
        w_down = din("w_down", [n_layers, NE, DFF, D]); b_down = din("b_down", [n_layers, NE, D])
    c_ident = din("c_ident", [P, P])
    c_trineg = din("c_trineg", [P, P])
    c_tri4 = din("c_tri4", [P, 4 * P])
    c_pow2 = din("c_pow2", [P, NBIS])
    c_rot = din("c_rot", [4, L, 256])
    y_out = nc.dram_tensor("y", [L, D], F32, kind="ExternalOutput").ap()
    x1_kind = "ExternalOutput" if dbg else "Internal"
    x1_d = nc.dram_tensor("x1_d", [L, D], F32, kind=x1_kind).ap()
    xa_d = nc.dram_tensor("xa_d", [L, D], F32, kind="Internal").ap()
    xb_d = nc.dram_tensor("xb_d", [L, D], F32, kind="Internal").ap()
    x1T_d = nc.dram_tensor("x1T_d", [D, L], BF16, kind="Internal").ap()
    macc_d = nc.dram_tensor("macc_d", [L, D], F32, kind="Internal").ap()

    gam = [1.0 - 2.0 ** (-5 - h) for h in range(4)]
    cdec = [g ** 128 for g in gam]

    with ExitStack() as es:
        S = Sched(nc, es)

        uid = [0]

        def sb(stack, name, shape, dt):
            uid[0] += 1
            return Buf(stack.enter_context(nc.sbuf_tensor(f"{name}_{uid[0]}", list(shape), dt)), name)

        pbig = [es.enter_context(nc.psum_tensor(f"pbig{i}", [P, 1024], F32)) for i in range(3)]
        psb = [Buf(pbig[i // 2][:, (i % 2) * 512:(i % 2 + 1) * 512], f"ps{i}") for i in range(6)]
        pst = [Buf(es.enter_context(nc.psum_tensor(f"pst{i}", [P, 1024], BF16)), f"pst{i}") for i in range(2)]
        rr = {"mm": 0, "acc": 0, "tr": 0}

        def ps_mm():
            rr["mm"] = (rr["mm"] + 1) % 4
            return psb[rr["mm"]]

        def ps_acc():
            rr["acc"] = (rr["acc"] + 1) % 2
            return psb[4 + rr["acc"]]

        def ps_tr():
            rr["tr"] = (rr["tr"] + 1) % 2
            return pst[rr["tr"]]

        ident = sb(es, "ident", [P, P], BF16)
        trineg = sb(es, "trineg", [P, P], F32)
        tri4 = sb(es, "tri4", [P, 4 * P], BF16)
        pow2 = sb(es, "pow2", [P, NBIS], F32)
        ones_bf = sb(es, "ones_bf", [P, P], BF16)
        S.dma("pool", ident[:], c_ident[:, :], writes=[ident])
        S.dma("pool", tri4[:], c_tri4[:, :], writes=[tri4])
        S.dma("sp", trineg[:], c_trineg[:, :], writes=[trineg])
        S.dma("sp", pow2[:], c_pow2[:, :], writes=[pow2])
        S.op("dve", lambda e: e.memset(ones_bf[:], 1.0), writes=[ones_bf])

        def dtiles(ap):
            return [Buf(ap[t * P:(t + 1) * P, :]) for t in range(NT)]
        xa_t = dtiles(xa_d); xb_t = dtiles(xb_d); x1_t = dtiles(x1_d); macc_t = dtiles(macc_d); y_t = dtiles(y_out)
        xin_t = dtiles(x_in)
        x1T_g = [Buf(x1T_d[:, g * GW:(g + 1) * GW]) for g in range(NG)]

        def layer_norm_tile(zb, z, gb, g_bc, bb, b_bc, scr):
            st, junk, jap = scr["st"], scr["junk"], scr["junk_ap"]
            S.op("dve", lambda e: e.tensor_reduce(st[:, 0:1], z, AX.X, ALU.add), reads=[zb], writes=[st])
            S.op("dve", lambda e: e.tensor_scalar(st[:, 1:2], st[:, 0:1], -1.0 / D, None, op0=ALU.mult), reads=[st], writes=[st])
            S.op("dve", lambda e: e.tensor_scalar(z, z, st[:, 1:2], None, op0=ALU.add), reads=[zb, st], writes=[zb])
            S.op("act", lambda e: e.activation(jap, z, AF.Square, accum_out=st[:, 2:3]), reads=[zb, st], writes=[junk, st])
            S.op("dve", lambda e: e.tensor_scalar(st[:, 3:4], st[:, 2:3], 1.0 / D, EPS, op0=ALU.mult, op1=ALU.add), reads=[st], writes=[st])
            S.op("act", lambda e: e.sqrt(st[:, 4:5], st[:, 3:4]), reads=[st], writes=[st])
            S.op("dve", lambda e: e.reciprocal(st[:, 4:5], st[:, 4:5]), reads=[st], writes=[st])
            S.op("dve", lambda e: e.scalar_tensor_tensor(z, z, st[:, 4:5], g_bc, op0=ALU.mult, op1=ALU.mult), reads=[zb, st, gb], writes=[zb])
            S.op("dve", lambda e: e.tensor_tensor(z, z, b_bc, ALU.add), reads=[zb, bb], writes=[zb])

        for l in range(n_layers):
            src_t = xin_t if l == 0 else (xa_t if l % 2 == 1 else xb_t)
            src_d = x_in if l == 0 else (xa_d if l % 2 == 1 else xb_d)
            last = (l == n_layers - 1)
            if do_moe:
                dst_t = y_t if last else (xa_t if l % 2 == 0 else xb_t)
                dst_d = y_out if last else (xa_d if l % 2 == 0 else xb_d)
            else:
                dst_t = None

            with ExitStack() as ms:
                kT = sb(ms, "kT", [P, 2, L], BF16)
                Vp = sb(ms, "Vp", [P, NT, 4, 65], BF16)
                kiT = sb(ms, "kiT", [P, L], BF16)
                state_f = sb(ms, "state_f", [P, 4, P], F32)
                state_b = sb(ms, "state_b", [P, 4, P], BF16)
                cw = sb(ms, "cw", [P, 4, 3], F32)
                hal = sb(ms, "hal", [P, 4, 2], F32)
                uc = sb(ms, "uc", [P, 2 + GW], F32)
                convT = sb(ms, "convT", [P, 4, GW], BF16)
                wbuf = [sb(ms, f"wbuf{i}", [P, 16, 512], BF16) for i in range(2)]
                xg = sb(ms, "xg", [P, GT, D], F32)
                xT = sb(ms, "xT", [P, 16, GW], BF16)
                qT = sb(ms, "qT", [P, 8, GW], BF16)
                qiT = sb(ms, "qiT", [P, 4, GW], BF16)
                vv = sb(ms, "vv", [P, GT, 512], BF16)
                sgt = sb(ms, "sgt", [P, GT, 512], BF16)
                iwt = sb(ms, "iwt", [P, GT, 8], F32)
                rotb = [sb(ms, "rotb0", [P, 2, 256], F32)] * 2
                rt = [sb(ms, f"rt{i}", [P, 4, 64], F32) for i in range(2)]
                qpk = sb(ms, "qpk", [P, GT, 2, 512], BF16)
                qkT = sb(ms, "qkT", [P, 2, 4, P], BF16)
                sdT = sb(ms, "sdT", [P, 4, P], BF16)
                osb = sb(ms, "osb", [P, 4, P], F32)
                cbj_ap = osb[:].rearrange("p h t -> p (h t)")
                sm = sb(ms, "sm", [P, 16], F32)
                retk = sb(ms, "retk", [P, 512], BF16)
                sc = sb(ms, "sc", [P, L], F32)
                rl = [sb(ms, f"rl{i}", [P, 512], F32) for i in range(2)]
                ccT = rl[1]
                mskT = sb(ms, "mskT", [P, NT, P], BF16)
                mblk = sb(ms, "mblk", [P, 8 * P], BF16)
                bis = sb(ms, "bis", [P, 8], F32)
                Wt = sb(ms, "Wt", [P, NBIS], F32)
                cnt = sb(ms, "cnt", [P, NBIS], F32)
                Eb = [sb(ms, f"Eb{i}", [P, 1024], BF16) for i in range(2)]
                rden = sb(ms, "rden", [P, 4], F32)
                atk = sb(ms, "atk", [P, 1024], BF16)
                lnst = sb(ms, "lnst", [P, 8], F32)
                mix_bufs = [kT, Vp, kiT, state_f, state_b, cw, hal, uc, convT, xg, xT, qT, qiT, vv, sgt,
                            iwt, qpk, qkT, sdT, osb, sm, retk, sc, mskT, mblk, bis, Wt, cnt, rden, atk, lnst] \
                    + wbuf + rt + rl + Eb + rotb[:1]
                mflat = mskT[:].rearrange("p k t -> p (k t)")
                xbf_ap = mflat[:, 0:D]
                concatT = xT

                for k in range(3):
                    S.dma("sp", cw[:, :, k], conv_w[l, k].rearrange("(cc p) -> p cc", p=P), writes=[cw], allow_slow_non_contiguous=True)
                S.op("dve", lambda e: e.memset(state_f[:], 0.0), writes=[state_f])
                S.op("dve", lambda e: e.memset(state_b[:], 0.0), writes=[state_b])
                S.op("dve", lambda e: e.memset(hal[:], 0.0), writes=[hal])
                S.op("dve", lambda e: e.memset(Vp[:], 1.0), writes=[Vp])
                ri = [0]

                wi = [0]

                def load_w(src_ap, ncols, rows_kc=16):
                    wb = wbuf[wi[0] % 2]
                    wi[0] += 1
                    S.dma("pool", wb[:, 0:rows_kc, 0:ncols], src_ap.rearrange("(kc p) n -> p kc n", p=P), writes=[wb])
                    return wb

                for tg in range(NG):
                    t0 = tg * GT
                    c0 = tg * GW
                    for i in range(GT):
                        S.dma("sp", xg[:, i, :], src_d[(t0 + i) * P:(t0 + i + 1) * P, :], reads=[src_t[t0 + i]], writes=[xg])
                    for i in range(GT):
                        S.op("act", lambda e: e.copy(xbf_ap, xg[:, i, :]), reads=[xg], writes=[mskT])
                        for half in range(2):
                            pt = ps_tr()
                            for k in range(8):
                                kc = half * 8 + k
                                S.op("pe", lambda e: e.transpose(pt[:, k * P:(k + 1) * P], xbf_ap[:, kc * P:(kc + 1) * P], ident[:]),
                                     reads=[mskT, ident], writes=[pt])
                            S.op("dve", lambda e: e.tensor_copy(
                                xT[:, half * 8:(half + 1) * 8, i * P:(i + 1) * P],
                                pt[:].rearrange("p (k t) -> p k t", k=8)), reads=[pt], writes=[xT])
                    for fg in range(7):
                        nch = min(4, NFC - fg * 4)
                        wb = load_w(w_in[l, :, fg * 512: fg * 512 + nch * 128], nch * 128)
                        for j in range(nch):
                            fc = fg * 4 + j
                            ps = ps_mm()
                            for kc in range(16):
                                S.op("pe", lambda e: e.matmul(ps[:], lhsT=wb[:, kc, j * P:(j + 1) * P], rhs=xT[:, kc, :],
                                                              start=(kc == 0), stop=(kc == 15)), reads=[wb, xT], writes=[ps])
                            if fc < 8:
                                S.op("act", lambda e: e.activation(qT[:, fc, :], ps[:], AF.Copy, scale=0.125), reads=[ps], writes=[qT])
                            elif fc < 10:
                                S.op("act", lambda e: e.copy(kT[:, fc - 8, c0:c0 + GW], ps[:]), reads=[ps], writes=[kT])
                            elif fc < 14:
                                S.op("dve", lambda e: e.tensor_copy(qiT[:, fc - 10, :], ps[:]), reads=[ps], writes=[qiT])
                            elif fc == 14:
                                S.op("act", lambda e: e.copy(kiT[:, c0:c0 + GW], ps[:]), reads=[ps], writes=[kiT])
                            else:
                                cj, kind = divmod(fc - 15, 3)
                                if kind == 0:
                                    S.op("act", lambda e: e.copy(cbj_ap, ps[:]), reads=[ps], writes=[osb])
                                elif kind == 1:
                                    S.op("act", lambda e: e.copy(uc[:, 2:2 + GW], ps[:]), reads=[ps], writes=[uc])
                                    S.op("dve", lambda e: e.tensor_copy(uc[:, 0:2], hal[:, cj, :]), reads=[hal], writes=[uc])
                                else:
                                    S.op("dve", lambda e: e.tensor_tensor(uc[:, 2:2 + GW], uc[:, 2:2 + GW], ps[:], ALU.mult), reads=[ps, uc], writes=[uc])
                                    S.op("dve", lambda e: e.tensor_copy(hal[:, cj, :], uc[:, GW:GW + 2]), reads=[uc], writes=[hal])
                                    S.op("dve", lambda e: e.tensor_scalar(ccT[:], uc[:, 2:2 + GW], cw[:, cj, 2:3], None, op0=ALU.mult),
                                         reads=[uc, cw], writes=[ccT])
                                    S.op("dve", lambda e: e.scalar_tensor_tensor(ccT[:], uc[:, 1:1 + GW], cw[:, cj, 1:2], ccT[:], op0=ALU.mult, op1=ALU.add),
                                         reads=[uc, cw, ccT], writes=[ccT])
                                    S.op("dve", lambda e: e.scalar_tensor_tensor(ccT[:], uc[:, 0:GW], cw[:, cj, 0:1], ccT[:], op0=ALU.mult, op1=ALU.add),
                                         reads=[uc, cw, ccT], writes=[ccT])
                                    S.op("dve", lambda e: e.tensor_tensor(convT[:, cj, :], ccT[:], cbj_ap, ALU.mult), reads=[ccT, osb], writes=[convT])
                    tm_groups = [(0, 512), (512, 512), (1024, 512), (1536, 512), (2048, 264)]
                    for gi, (tc0, tn) in enumerate(tm_groups):
                        wb = load_w(w_in[l, :, FCOLS + tc0: FCOLS + tc0 + tn], tn)
                        for i in range(GT):
                            p = t0 + i
                            if gi < 2:
                                rb_ = rotb[ri[0] % 2]
                                ri[0] += 1
                                S.dma("sp", rb_[:], c_rot[2 * gi:2 * gi + 2, p * P:(p + 1) * P, :].rearrange("w p c -> p w c"), writes=[rb_])
                            ps = ps_mm()
                            for kc in range(16):
                                S.op("pe", lambda e: e.matmul(ps[:, 0:tn], lhsT=xT[:, kc, i * P:(i + 1) * P], rhs=wb[:, kc, 0:tn],
                                                              start=(kc == 0), stop=(kc == 15)), reads=[wb, xT], writes=[ps])
                            if gi < 2:
                                xv = ps[:].rearrange("p (h two d) -> p h two d", two=2, d=64)
                                x1v, x2v = xv[:, :, 0, :], xv[:, :, 1, :]
                                C = rb_[:, 0, :].rearrange("p (h d) -> p h d", d=64)
                                Sn = rb_[:, 1, :].rearrange("p (h d) -> p h d", d=64)
                                ov = qpk[:, i, gi, :].rearrange("p (h two d) -> p h two d", two=2, d=64)
                                S.op("dve", lambda e: e.tensor_tensor(rt[0][:], x1v, C, ALU.mult), reads=[ps, rb_], writes=[rt[0]])
                                S.op("dve", lambda e: e.tensor_tensor(rt[1][:], x2v, Sn, ALU.mult), reads=[ps, rb_], writes=[rt[1]])
                                S.op("dve", lambda e: e.tensor_tensor(ov[:, :, 0, :], rt[0][:], rt[1][:], ALU.subtract), reads=[rt[0], rt[1]], writes=[qpk])
                                S.op("dve", lambda e: e.tensor_tensor(rt[0][:], x1v, Sn, ALU.mult), reads=[ps, rb_], writes=[rt[0]])
                                S.op("dve", lambda e: e.tensor_tensor(rt[1][:], x2v, C, ALU.mult), reads=[ps, rb_], writes=[rt[1]])
                                S.op("dve", lambda e: e.tensor_tensor(ov[:, :, 1, :], rt[0][:], rt[1][:], ALU.add), reads=[rt[0], rt[1]], writes=[qpk])
                            elif gi == 2:
                                S.op("act", lambda e: e.copy(vv[:, i, :], ps[:]), reads=[ps], writes=[vv])
                            elif gi == 3:
                                S.op("act", lambda e: e.activation(sgt[:, i, :], ps[:], AF.Silu), reads=[ps], writes=[sgt])
                            else:
                                S.op("dve", lambda e: e.tensor_copy(
                                    Vp[:, p, :, 0:64], ps[:, 0:256].rearrange("p (g d) -> p g d", g=4)), reads=[ps], writes=[Vp])
                                S.op("dve", lambda e: e.tensor_scalar(iwt[:, i, :], ps[:, 256:264], 512.0 ** -0.5, None, op0=ALU.mult),
                                     reads=[ps], writes=[iwt])
                    S.barrier([xT])
                    for cj in range(4):
                        S.op("act", lambda e: e.copy(concatT[:, 8 + cj, :], convT[:, cj, :]), reads=[convT], writes=[concatT])

                    for i in range(GT):
                        p = t0 + i
                        tcol = slice(i * P, (i + 1) * P)
                        pt = ps_tr()
                        for which in range(2):
                            for h in range(4):
                                S.op("pe", lambda e: e.transpose(pt[:, (which * 4 + h) * P:(which * 4 + h + 1) * P],
                                                                 qpk[:, i, which, h * P:(h + 1) * P], ident[:]), reads=[qpk, ident], writes=[pt])
                        S.op("act", lambda e: e.copy(qkT[:].rearrange("p a h t -> p (a h t)"), pt[:]), reads=[pt], writes=[qkT])
                        ps = ps_mm()
                        for h in range(4):
                            S.op("pe", lambda e: e.matmul(ps[:, h * P:(h + 1) * P], lhsT=qkT[:, 1, h, :], rhs=qkT[:, 0, h, :], start=True, stop=True),
                                 reads=[qkT], writes=[ps])
                        S.op("dve", lambda e: e.tensor_tensor(sdT[:].rearrange("p h t -> p (h t)"), ps[:], tri4[:], ALU.mult), reads=[ps, tri4], writes=[sdT])
                        po = ps_acc()
                        for h in range(4):
                            S.op("pe", lambda e: e.matmul(po[:, h * P:(h + 1) * P], lhsT=sdT[:, h, :], rhs=vv[:, i, h * P:(h + 1) * P], start=True, stop=False),
                                 reads=[sdT, vv], writes=[po])
                            S.op("pe", lambda e: e.matmul(po[:, h * P:(h + 1) * P], lhsT=qkT[:, 0, h, :], rhs=state_b[:, h, :], start=False, stop=True),
                                 reads=[qkT, state_b], writes=[po])
                        pa = ps_mm()
                        for h in range(4):
                            S.op("pe", lambda e: e.matmul(pa[:, h * P:(h + 1) * P], lhsT=qpk[:, i, 1, h * P:(h + 1) * P], rhs=vv[:, i, h * P:(h + 1) * P], start=True, stop=True),
                                 reads=[qpk, vv], writes=[pa])
                        S.op("dve", lambda e: e.tensor_tensor(state_f[:].rearrange("p h t -> p (h t)"), state_f[:].rearrange("p h t -> p (h t)"), pa[:], ALU.add),
                             reads=[pa, state_f], writes=[state_f])
                        for h in range(4):
                            S.op("dve", lambda e: e.tensor_scalar(state_f[:, h, :], state_f[:, h, :], float(cdec[h]), None, op0=ALU.mult),
                                 reads=[state_f], writes=[state_f])
                        S.op("act", lambda e: e.copy(state_b[:], state_f[:]), reads=[state_f], writes=[state_b])
                        osq = rl[0]
                        S.op("act", lambda e: e.copy(osb[:].rearrange("p h t -> p (h t)"), po[:]), reads=[po], writes=[osb])
                        S.op("dve", lambda e: e.tensor_reduce(sm[:, 0:4], osb[:], AX.X, ALU.add), reads=[osb], writes=[sm])
                        S.op("dve", lambda e: e.tensor_tensor(osq[:], osb[:].rearrange("p h t -> p (h t)"), osb[:].rearrange("p h t -> p (h t)"), ALU.mult), reads=[osb], writes=[osq])
                        S.op("dve", lambda e: e.tensor_reduce(sm[:, 4:8], osq[:].rearrange("p (h t) -> p h t", h=4), AX.X, ALU.add), reads=[osq], writes=[sm])
                        S.op("dve", lambda e: e.tensor_scalar(sm[:, 0:4], sm[:, 0:4], 1.0 / 128, None, op0=ALU.mult), reads=[sm], writes=[sm])
                        S.op("dve", lambda e: e.tensor_tensor(sm[:, 8:12], sm[:, 0:4], sm[:, 0:4], ALU.mult), reads=[sm], writes=[sm])
                        S.op("dve", lambda e: e.scalar_tensor_tensor(sm[:, 12:16], sm[:, 4:8], 1.0 / 128, sm[:, 8:12], op0=ALU.mult, op1=ALU.subtract),
                             reads=[sm], writes=[sm])
                        S.op("dve", lambda e: e.tensor_scalar(sm[:, 12:16], sm[:, 12:16], EPS, None, op0=ALU.add), reads=[sm], writes=[sm])
                        S.op("act", lambda e: e.sqrt(sm[:, 12:16], sm[:, 12:16]), reads=[sm], writes=[sm])
                        S.op("dve", lambda e: e.reciprocal(sm[:, 12:16], sm[:, 12:16]), reads=[sm], writes=[sm])
                        for h in range(4):
                            S.op("dve", lambda e: e.tensor_scalar(osb[:, h, :], osb[:, h, :], sm[:, h:h + 1], sm[:, 12 + h:13 + h], op0=ALU.subtract, op1=ALU.mult),
                                 reads=[osb, sm], writes=[osb])
                        S.op("dve", lambda e: e.tensor_tensor(retk[:], osb[:].rearrange("p h t -> p (h t)"), sgt[:, i, :], ALU.mult), reads=[osb, sgt], writes=[retk])
                        pt = ps_tr()
                        for h in range(4):
                            S.op("pe", lambda e: e.transpose(pt[:, h * P:(h + 1) * P], retk[:, h * P:(h + 1) * P], ident[:]), reads=[retk, ident], writes=[pt])
                        S.op("act", lambda e: e.copy(concatT[:, 12:16, tcol], pt[:, 0:512].rearrange("p (h t) -> p h t", h=4)), reads=[pt], writes=[concatT])

                        nk = (p + 1) * P
                        for cs in range(0, nk, 512):
                            cn = min(512, nk - cs)
                            for h in range(8):
                                ps = ps_mm()
                                hs = slice((h % 2) * 64, (h % 2) * 64 + 64)
                                S.op("pe", lambda e: e.matmul(ps[:, 0:cn], lhsT=qiT[hs, h // 2, tcol], rhs=kiT[hs, cs:cs + cn], start=True, stop=True),
                                     reads=[qiT, kiT], writes=[ps])
                                r = rl[h % 2]
                                S.op("act", lambda e: e.activation(r[:, 0:cn], ps[:, 0:cn], AF.Relu), reads=[ps], writes=[r])
                                if h == 0:
                                    S.op("dve", lambda e: e.tensor_scalar(sc[:, cs:cs + cn], r[:, 0:cn], iwt[:, i, 0:1], None, op0=ALU.mult),
                                         reads=[r, iwt], writes=[sc])
                                else:
                                    S.op("dve", lambda e: e.scalar_tensor_tensor(sc[:, cs:cs + cn], r[:, 0:cn], iwt[:, i, h:h + 1], sc[:, cs:cs + cn],
                                                                                 op0=ALU.mult, op1=ALU.add), reads=[r, iwt, sc], writes=[sc])
                        S.op("dve", lambda e: e.tensor_reduce(bis[:, 0:1], sc[:, 0:nk], AX.X, ALU.min), reads=[sc], writes=[bis])
                        S.op("dve", lambda e: e.tensor_reduce(bis[:, 1:2], sc[:, 0:nk], AX.X, ALU.max), reads=[sc], writes=[bis])
                        S.op("dve", lambda e: e.tensor_tensor(bis[:, 2:3], bis[:, 1:2], bis[:, 0:1], ALU.subtract), reads=[bis], writes=[bis])
                        S.op("dve", lambda e: e.tensor_scalar(Wt[:], pow2[:], bis[:, 2:3], None, op0=ALU.mult), reads=[bis, pow2], writes=[Wt])
                        S.op("dve", lambda e: e.tensor_tensor(sc[:, p * P:(p + 1) * P], sc[:, p * P:(p + 1) * P], trineg[:], ALU.add), reads=[sc, trineg], writes=[sc])
                        S.op("dve", lambda e: e.memset(cnt[:], 0.0), writes=[cnt])
                        if nk > TOPK:
                            for n in range(NBIS):
                                S.op("dve", lambda e: e.tensor_tensor(bis[:, 3:4], bis[:, 0:1], Wt[:, n:n + 1], ALU.add), reads=[bis, Wt], writes=[bis])
                                S.op("dve", lambda e: e.tensor_scalar(mflat[:, 0:nk], sc[:, 0:nk], bis[:, 3:4], 0.0, op0=ALU.is_ge, op1=ALU.add,
                                                                      accum_out=cnt[:, n:n + 1]), reads=[sc, bis, cnt], writes=[mskT, cnt])
                                S.op("dve", lambda e: e.tensor_scalar(bis[:, 4:5], cnt[:, n:n + 1], TOPK - 0.5, Wt[:, n:n + 1], op0=ALU.is_ge, op1=ALU.mult),
                                     reads=[cnt, Wt], writes=[bis])
                                S.op("dve", lambda e: e.tensor_tensor(bis[:, 0:1], bis[:, 0:1], bis[:, 4:5], ALU.add), reads=[bis], writes=[bis])
                        for b0 in range(0, p + 1, 8):
                            nb = min(8, p + 1 - b0)
                            S.op("dve", lambda e: e.tensor_scalar(mblk[:, 0:nb * P], sc[:, b0 * P:(b0 + nb) * P], bis[:, 0:1], None, op0=ALU.is_ge),
                                 reads=[sc, bis], writes=[mblk])
                            pt = ps_tr()
                            for k in range(nb):
                                S.op("pe", lambda e: e.transpose(pt[:, k * P:(k + 1) * P], mblk[:, k * P:(k + 1) * P], ident[:]),
                                     reads=[mblk, ident], writes=[pt])
                            S.op("act", lambda e: e.copy(mskT[:, b0:b0 + nb, :], pt[:, 0:nb * P].rearrange("p (k t) -> p k t", k=nb)), reads=[pt], writes=[mskT])
                        ei = 0
                        for gp in range(2):
                            accs = [psb[4], psb[5]]
                            for kb in range(p + 1):
                                bj = ei % 2
                                big_ap = pbig[bj]
                                hb = [psb[2 * bj], psb[2 * bj + 1]]
                                for hf in range(2):
                                    hs = slice(hf * 64, hf * 64 + 64)
                                    for r4 in range(4):
                                        c_ = (hf * 4 + r4) * P
                                        S.op("pe", lambda e: e.matmul(big_ap[:, c_:c_ + P], lhsT=kT[hs, gp, kb * P:(kb + 1) * P], rhs=qT[hs, gp * 4 + r4, tcol],
                                                                      start=True, stop=True), reads=[kT, qT], writes=[hb[hf]])
                                E = Eb[ei % 2]
                                ei += 1
                                S.op("act", lambda e: e.activation(E[:], big_ap[:, :], AF.Exp), reads=hb, writes=[E])
                                S.op("dve", lambda e: e.tensor_tensor(E[:].rearrange("p (r t) -> p r t", r=8), E[:].rearrange("p (r t) -> p r t", r=8),
                                                                      mskT[:, kb:kb + 1, :].to_broadcast([P, 8, P]), ALU.mult), reads=[E, mskT], writes=[E])
                                for hf in range(2):
                                    for r4 in range(4):
                                        c_ = (hf * 4 + r4) * P
                                        S.op("pe", lambda e: e.matmul(accs[hf][:, r4 * P:r4 * P + 65], lhsT=E[:, c_:c_ + P], rhs=Vp[:, kb, 2 * gp + hf, :],
                                                                      start=(kb == 0), stop=(kb == p)), reads=[E, Vp], writes=[accs[hf]])
                            for hf in range(2):
                                g = 2 * gp + hf
                                po = accs[hf]
                                pov = po[:].rearrange("p (r c) -> p r c", r=4)
                                S.op("dve", lambda e: e.reciprocal(rden[:], pov[:, :, 64]), reads=[po], writes=[rden])
                                for r4 in range(4):
                                    hh = g * 4 + r4
                                    S.op("dve", lambda e: e.tensor_scalar(atk[:, hh * 64:(hh + 1) * 64], po[:, r4 * P:r4 * P + 64], rden[:, r4:r4 + 1], None, op0=ALU.mult),
                                         reads=[po, rden], writes=[atk])
                        pt = ps_tr()
                        for k in range(8):
                            S.op("pe", lambda e: e.transpose(pt[:, k * P:(k + 1) * P], atk[:, k * P:(k + 1) * P], ident[:]), reads=[atk, ident], writes=[pt])
                        S.op("act", lambda e: e.copy(concatT[:, 0:8, tcol], pt[:].rearrange("p (k t) -> p k t", k=8)), reads=[pt], writes=[concatT])

                    S.dma("sp", sc[:, 0:D], ln1_g[l:l + 1, :].to_broadcast([P, D]), writes=[sc])
                    S.dma("sp", sc[:, D:2 * D], ln1_b[l:l + 1, :].to_broadcast([P, D]), writes=[sc])
                    for cg in range(4):
                        wb = load_w(w_out[l, :, cg * 512:(cg + 1) * 512], 512)
                        for i in range(GT):
                            ps = ps_mm()
                            for kc in range(16):
                                S.op("pe", lambda e: e.matmul(ps[:], lhsT=concatT[:, kc, i * P:(i + 1) * P], rhs=wb[:, kc, :], start=(kc == 0), stop=(kc == 15)),
                                     reads=[wb, concatT], writes=[ps])
                            S.op("dve", lambda e: e.scalar_tensor_tensor(xg[:, i, cg * 512:(cg + 1) * 512], xg[:, i, cg * 512:(cg + 1) * 512], float(ALPHA), ps[:],
                                                                         op0=ALU.mult, op1=ALU.add), reads=[ps, xg], writes=[xg])
                    S.barrier([concatT])
                    for i in range(GT):
                        p = t0 + i
                        layer_norm_tile(xg, xg[:, i, :], sc, sc[:, 0:D], sc, sc[:, D:2 * D], {"st": lnst, "junk": mskT, "junk_ap": mflat[:, 0:D]})
                        S.dma("sp", x1_d[p * P:(p + 1) * P, :], xg[:, i, :], reads=[xg], writes=[x1_t[p]])
                        if do_moe:
                            S.op("act", lambda e: e.copy(xbf_ap, xg[:, i, :]), reads=[xg], writes=[mskT])
                            for half in range(2):
                                pt = ps_tr()
                                for k in range(8):
                                    kc = half * 8 + k
                                    S.op("pe", lambda e: e.transpose(pt[:, k * P:(k + 1) * P], xbf_ap[:, kc * P:(kc + 1) * P], ident[:]), reads=[mskT, ident], writes=[pt])
                                S.op("dve", lambda e: e.tensor_copy(concatT[:, half * 8:(half + 1) * 8, i * P:(i + 1) * P],
                                                                    pt[:].rearrange("p (k t) -> p k t", k=8)), reads=[pt], writes=[concatT])
                    if do_moe:
                        S.dma("sp", x1T_d[:, c0:c0 + GW].rearrange("(kc p) t -> p kc t", p=P), concatT[:], reads=[concatT], writes=[x1T_g[tg]])
                    S.barrier([concatT])
                S.barrier(mix_bufs)

            if not do_moe:
                continue
            with ExitStack() as ms:
                wgus = [sb(ms, f"wgu{i}", [P, 16, 2 * DFF], BF16) for i in range(2)]
                wd = sb(ms, "wd", [P, 6, D], BF16)
                bgu = sb(ms, "bgu", [P, NE, 6, 2], F32)
                bd = sb(ms, "bd", [1, D], BF16)
                rw = sb(ms, "rw", [P, 16, NE], BF16)
                rb = sb(ms, "rb", [1, NE], BF16)
                xt = [sb(ms, f"xt{i}", [P, 16, GW], BF16) for i in range(2)]
                comb = sb(ms, "comb", [P, NT, NE], F32)
                lg = sb(ms, "lg", [P, NE], F32)
                mx8 = sb(ms, "mx8", [P, 8], F32)
                rs = sb(ms, "rs", [P, 4], F32)
                actT = sb(ms, "actT", [P, 6, GW], BF16)
                tgs = [sb(ms, f"tg{i}", [P, GW], F32) for i in range(2)]
                tus = [sb(ms, f"tu{i}", [P, GW], F32) for i in range(2)]
                tss = [sb(ms, f"ts{i}", [P, GW], F32) for i in range(2)]
                stg = [sb(ms, f"stg{i}", [P, D], F32) for i in range(2)]
                lnst = sb(ms, "lnst2", [P, 8], F32)
                moe_bufs = [wd, bgu, bd, rw, rb, comb, lg, mx8, rs, actT, lnst] + xt + stg + wgus + tgs + tus + tss

                S.dma("pool", rw[:], router_w[l].rearrange("(kc p) n -> p kc n", p=P), writes=[rw])
                S.dma("pool", rb[:], router_b[l:l + 1, :], writes=[rb])
                for e_ in range(NE):
                    S.dma("sp", bgu[:, e_, :, :], b_gu[l, e_].rearrange("(fc p two) -> p fc two", p=P, two=2), writes=[bgu], allow_slow_non_contiguous=True)

                xi = [0]

                def load_xt(g):
                    b = xt[xi[0] % 2]
                    xi[0] += 1
                    S.dma("sp", b[:], x1T_d[:, g * GW:(g + 1) * GW].rearrange("(kc p) t -> p kc t", p=P), reads=[x1T_g[g]], writes=[b])
                    return b

                def load_wgu(ex_):
                    wb_ = wgus[ex_ % 2]
                    for kq in range(4):
                        S.dma("pool", wb_[:, kq * 4:(kq + 1) * 4, :], w_gu[l, ex_, kq * 512:(kq + 1) * 512, :].rearrange("(kc p) n -> p kc n", p=P), writes=[wb_])

                pre = set()

                def acc_load(tt_):
                    sb_ = stg[tt_ % 2]
                    S.dma("sp", sb_[:], macc_d[tt_ * P:(tt_ + 1) * P, :], reads=[macc_t[tt_]], writes=[sb_])

                load_wgu(0)
                nxt = load_xt(0)
                fi = 0
                for ex in range(NE):
                    wgu = wgus[ex % 2]
                    if ex + 1 < NE:
                        load_wgu(ex + 1)
                    S.dma("pool", wd[:], w_down[l, ex].rearrange("(fc p) n -> p fc n", p=P), writes=[wd])
                    S.dma("pool", bd[:], b_down[l, ex:ex + 1, :], writes=[bd])
                    for g in range(NG):
                        xb_ = nxt
                        if not (ex == NE - 1 and g == NG - 1):
                            nxt = load_xt((g + 1) % NG)
                        if ex == 0:
                            for i in range(GT):
                                tt = g * GT + i
                                ps = ps_mm()
                                for kc in range(16):
                                    S.op("pe", lambda e: e.matmul(ps[:, 0:NE], lhsT=xb_[:, kc, i * P:(i + 1) * P], rhs=rw[:, kc, :], start=(kc == 0), stop=False),
                                         reads=[xb_, rw], writes=[ps])
                                S.op("pe", lambda e: e.matmul(ps[:, 0:NE], lhsT=ones_bf[0:1, :], rhs=rb[0:1, :], start=False, stop=True), reads=[ones_bf, rb], writes=[ps])
                                S.op("act", lambda e: e.copy(lg[:], ps[:, 0:NE]), reads=[ps], writes=[lg])
                                S.op("dve", lambda e: e.max(out=mx8[:], in_=lg[:]), reads=[lg], writes=[mx8])
                                S.op("dve", lambda e: e.tensor_scalar(rs[:, 0:1], mx8[:, 0:1], -1.0, None, op0=ALU.mult), reads=[mx8], writes=[rs])
                                S.op("act", lambda e: e.activation(comb[:, tt, :], lg[:], AF.Exp, bias=rs[:, 0:1], scale=1.0), reads=[lg, rs], writes=[comb])
                                S.op("dve", lambda e: e.tensor_scalar(lg[:], lg[:], mx8[:, 3:4], None, op0=ALU.is_ge), reads=[lg, mx8], writes=[lg])
                                S.op("dve", lambda e: e.tensor_tensor(comb[:, tt, :], comb[:, tt, :], lg[:], ALU.mult), reads=[comb, lg], writes=[comb])
                                S.op("dve", lambda e: e.tensor_reduce(rs[:, 1:2], comb[:, tt, :], AX.X, ALU.add), reads=[comb], writes=[rs])
                                S.op("dve", lambda e: e.reciprocal(rs[:, 2:3], rs[:, 1:2]), reads=[rs], writes=[rs])
                                S.op("dve", lambda e: e.tensor_scalar(comb[:, tt, :], comb[:, tt, :], rs[:, 2:3], None, op0=ALU.mult), reads=[comb, rs], writes=[comb])
                        for fc in range(6):
                            tg_, tu_, ts_ = tgs[fi % 2], tus[fi % 2], tss[fi % 2]
                            fi += 1
                            pg_ = ps_mm()
                            for kc in range(16):
                                S.op("pe", lambda e: e.matmul(pg_[:], lhsT=wgu[:, kc, fc * 256: fc * 256 + 256: 2], rhs=xb_[:, kc, :], start=(kc == 0), stop=(kc == 15)),
                                     reads=[wgu, xb_], writes=[pg_])
                            pu_ = ps_mm()
                            for kc in range(16):
                                S.op("pe", lambda e: e.matmul(pu_[:], lhsT=wgu[:, kc, fc * 256 + 1: fc * 256 + 256: 2], rhs=xb_[:, kc, :], start=(kc == 0), stop=(kc == 15)),
                                     reads=[wgu, xb_], writes=[pu_])
                            S.op("dve", lambda e: e.tensor_scalar(tg_[:], pg_[:], bgu[:, ex, fc, 0:1], 7.0, op0=ALU.add, op1=ALU.min), reads=[pg_, bgu], writes=[tg_])
                            S.op("act", lambda e: e.activation(ts_[:], tg_[:], AF.Sigmoid, scale=1.702), reads=[tg_], writes=[ts_])
                            S.op("dve", lambda e: e.tensor_scalar(tu_[:], pu_[:], bgu[:, ex, fc, 1:2], 7.0, op0=ALU.add, op1=ALU.min), reads=[pu_, bgu], writes=[tu_])
                            S.op("dve", lambda e: e.tensor_scalar(tu_[:], tu_[:], -7.0, 1.0, op0=ALU.max, op1=ALU.add), reads=[tu_], writes=[tu_])
                            S.op("dve", lambda e: e.tensor_tensor(tg_[:], tg_[:], ts_[:], ALU.mult), reads=[tg_, ts_], writes=[tg_])
                            S.op("dve", lambda e: e.tensor_tensor(actT[:, fc, :], tg_[:], tu_[:], ALU.mult), reads=[tg_, tu_], writes=[actT])
                        for i in range(GT):
                            tt = g * GT + i
                            st_ = stg[tt % 2]
                            if ex > 0 and (ex, tt) not in pre:
                                acc_load(tt)
                            nx = (ex, tt + 1) if tt + 1 < NT else (ex + 1, 0)
                            if 0 < nx[0] < NE:
                                acc_load(nx[1])
                                pre.add(nx)
                            for nq in range(4):
                                po = ps_acc()
                                for fc in range(6):
                                    S.op("pe", lambda e: e.matmul(po[:], lhsT=actT[:, fc, i * P:(i + 1) * P], rhs=wd[:, fc, nq * 512:(nq + 1) * 512], start=(fc == 0), stop=False),
                                         reads=[actT, wd], writes=[po])
                                S.op("pe", lambda e: e.matmul(po[:], lhsT=ones_bf[0:1, :], rhs=bd[0:1, nq * 512:(nq + 1) * 512], start=False, stop=True),
                                     reads=[ones_bf, bd], writes=[po])
                                if ex == 0:
                                    S.op("act", lambda e: e.activation(st_[:, nq * 512:(nq + 1) * 512], po[:], AF.Copy, scale=comb[:, tt, ex:ex + 1]),
                                         reads=[po, comb], writes=[st_])
                                else:
                                    S.op("dve", lambda e: e.scalar_tensor_tensor(st_[:, nq * 512:(nq + 1) * 512], po[:], comb[:, tt, ex:ex + 1], st_[:, nq * 512:(nq + 1) * 512],
                                                                                 op0=ALU.mult, op1=ALU.add), reads=[po, comb, st_], writes=[st_])
                            S.dma("sp", macc_d[tt * P:(tt + 1) * P, :], st_[:], reads=[st_], writes=[macc_t[tt]])
                g2, b2, lnj = wgus[0], wgus[0], wgus[1]
                g2f = wgus[0][:].rearrange("p a b -> p (a b)").bitcast(F32)
                S.dma("sp", g2f[:, 0:D], ln2_g[l:l + 1, :].to_broadcast([P, D]), writes=[g2])
                S.dma("sp", g2f[:, D:2 * D], ln2_b[l:l + 1, :].to_broadcast([P, D]), writes=[b2])
                lnj_ap = wgus[1][:].rearrange("p a b -> p (a b)")[:, 0:D]
                for tt in range(NT):
                    a_, b_ = stg[0], stg[1]
                    S.dma("sp", a_[:], x1_d[tt * P:(tt + 1) * P, :], reads=[x1_t[tt]], writes=[a_])
                    S.dma("sp", b_[:], macc_d[tt * P:(tt + 1) * P, :], reads=[macc_t[tt]], writes=[b_])
                    S.op("dve", lambda e: e.scalar_tensor_tensor(a_[:], a_[:], float(ALPHA), b_[:], op0=ALU.mult, op1=ALU.add), reads=[a_, b_], writes=[a_])
                    layer_norm_tile(a_, a_[:], g2, g2f[:, 0:D], b2, g2f[:, D:2 * D], {"st": lnst, "junk": lnj, "junk_ap": lnj_ap})
                    S.dma("sp", dst_d[tt * P:(tt + 1) * P, :], a_[:], reads=[a_], writes=[dst_t[tt]])
                S.barrier(moe_bufs)

        if not do_moe:
            with ExitStack() as ms:
                tmp = sb(ms, "cp", [P, D], F32)
                for tt in range(NT):
                    S.dma("sp", tmp[:], x1_d[tt * P:(tt + 1) * P, :], reads=[x1_t[tt]], writes=[tmp])
                    S.dma("sp", y_out[tt * P:(tt + 1) * P, :], tmp[:], reads=[tmp], writes=[y_t[tt]])
                S.barrier([tmp])
        S.drain_dmas("sp")
        print("instr counts", S.tot, "sems", S.nsem)
    return nc


def _col_perm():
    aq, ak, av, iq, ik, iw = 0, 1024, 1280, 1536, 2048, 2112
    cb, cc, ch, rq, rk, rv, rg = 2120, 2632, 3144, 3656, 4168, 4680, 5192
    idx = []
    for pg in range(2):
        for r in range(4):
            for g in (2 * pg, 2 * pg + 1):
                h = 4 * g + r
                idx += list(range(aq + h * 64, aq + h * 64 + 64))
    idx += list(range(ak, ak + 256))
    idx += list(range(iq, iq + 512))
    idx += list(range(ik, ik + 64)) * 2
    for j in range(4):
        idx += list(range(cb + j * 128, cb + (j + 1) * 128)) + list(range(cc + j * 128, cc + (j + 1) * 128)) \
            + list(range(ch + j * 128, ch + (j + 1) * 128))
    idx += list(range(rq, rq + 512)) + list(range(rk, rk + 512)) + list(range(rv, rv + 512)) + list(range(rg, rg + 512))
    idx += list(range(av, av + 256)) + list(range(iw, iw + 8))
    assert len(idx) == NCOL
    return np.asarray(idx)


def _consts():
    P = 128
    t = np.arange(P)
    trineg = np.where(t[None, :] <= t[:, None], 0.0, -1e30).astype(np.float32)
    triT = (t[None, :] >= t[:, None]).astype(np.float32)
    tri4 = np.tile(triT, (1, 4))
    pow2 = np.tile((2.0 ** -(np.arange(NBIS) + 1.0))[None, :], (P, 1)).astype(np.float32)
    half = 64
    inv_freq = (10000.0 ** (-np.linspace(0.0, 1.0, half, dtype=np.float32))).astype(np.float32)
    pos = np.arange(L, dtype=np.float32)
    ang = (pos[:, None] * inv_freq[None, :]).astype(np.float32).astype(np.float64)
    cos, sin = np.cos(ang), np.sin(ang)
    i = (np.arange(L) % 128).astype(np.float64)
    rot = np.zeros((4, L, 4, 64), np.float64)
    for h in range(4):
        lgm = np.log1p(-(2.0 ** (-5.0 - h)))
        qd = np.exp((i + 1.0) * lgm)[:, None]
        kd = (128.0 ** -0.5) * np.exp(-(i + 1.0) * lgm)[:, None]
        rot[0, :, h] = cos * qd; rot[1, :, h] = sin * qd
        rot[2, :, h] = cos * kd; rot[3, :, h] = sin * kd
    return {"c_ident": np.eye(P, dtype=np.float32), "c_trineg": trineg, "c_tri4": tri4.astype(np.float32),
            "c_pow2": pow2, "c_rot": rot.reshape(4, L, 256).astype(np.float32)}


_CACHE = {}


def _run(inputs, n_layers=DEPTH, do_moe=True, dbg=False, n_cores=2):
    key = (n_layers, do_moe, dbg)
    if key not in _CACHE:
        _CACHE[key] = build_program(n_layers, do_moe, dbg)
    nc = _CACHE[key]
    perm = _col_perm()
    f = lambda a: np.ascontiguousarray(np.asarray(a, dtype=np.float32))
    shared = {
        "w_in": f(np.asarray(inputs["w_in"])[:n_layers][:, :, perm]),
        "conv_w": f(inputs["conv_w"][:n_layers]), "w_out": f(inputs["w_out"][:n_layers]),
        "ln1_g": f(inputs["ln1_g"][:n_layers]), "ln1_b": f(inputs["ln1_b"][:n_layers]),
        "ln2_g": f(inputs["ln2_g"][:n_layers]), "ln2_b": f(inputs["ln2_b"][:n_layers]),
    }
    if do_moe:
        for k in ("router_w", "router_b", "w_gu", "b_gu", "w_down", "b_down"):
            shared[k] = f(inputs[k][:n_layers])
    shared.update(_consts())
    x = np.asarray(inputs["x"], dtype=np.float32)
    in_maps = [dict(shared, x=np.ascontiguousarray(x[b])) for b in range(n_cores)]
    res = run_bass_kernel_spmd(nc, in_maps, core_ids=list(range(n_cores)))
    return res


def kernel(**inputs):
    res = _run(inputs)
    return np.stack([res.results[b]["y"] for b in range(2)], axis=0).astype(np.float32)
```

```python
import numpy as np
from contextlib import ExitStack
import concourse.bass as bass
import concourse.mybir as mybir
from concourse.bass_utils import run_bass_kernel_spmd

F32 = mybir.dt.float32
BF16 = mybir.dt.bfloat16
ALU = mybir.AluOpType
AF = mybir.ActivationFunctionType
AX = mybir.AxisListType

D = 2048
L = 4096
NT = 32
DEPTH = 4
NE = 32
DFF = 768
ALPHA = (2 * DEPTH) ** 0.25
EPS = 1e-5
TOPK = 256
NBIS = 14
GT = 4
NG = NT // GT
GW = GT * 128
NFC = 27
FCOLS = NFC * 128
TCOLS = 2312
NCOL = FCOLS + TCOLS
EPOCH = 30000


class Buf:
    __slots__ = ("ap", "w", "r", "name")

    def __init__(self, ap, name=""):
        self.ap = ap
        self.w = None
        self.r = {}
        self.name = name

    def __getitem__(self, idx):
        return self.ap[idx]


class Sched:
    ENG = ("pe", "act", "dve", "pool", "sp")

    def __init__(self, nc, es, n_dma_sems=32):
        self.nc = nc
        self.es = es
        self.e = {"pe": nc.tensor, "act": nc.scalar, "dve": nc.vector, "pool": nc.gpsimd, "sp": nc.sync}
        self.sem = {}
        self.cnt = {}
        self.tot = {k: 0 for k in self.ENG}
        self.nsem = 0
        for k in self.ENG:
            self.sem[k] = self._newsem(k)
            self.cnt[k] = 0
        self.dma_sems = [[self._newsem("dma"), 0] for _ in range(n_dma_sems)]
        self.dma_rr = 0
        self.seen = {k: {} for k in self.ENG}

    def _newsem(self, name):
        self.nsem += 1
        return self.es.enter_context(self.nc.semaphore(f"{name}_{self.nsem}"))

    def _wait(self, eng, ev):
        if ev is None:
            return
        sem, val, src = ev
        if src == "pe" and eng == "pe":
            return
        s = self.seen[eng]
        if s.get(id(sem), 0) >= val:
            return
        self.e[eng].wait_ge(sem, val)
        s[id(sem)] = val

    def _deps(self, eng, reads, writes):
        for b in reads:
            self._wait(eng, b.w)
        for b in writes:
            self._wait(eng, b.w)
            for ev in b.r.values():
                self._wait(eng, ev)

    def _mark(self, ev, reads, writes, key):
        for b in reads:
            b.r[key] = ev
        for b in writes:
            b.w = ev
            b.r = {}

    def op(self, eng, fn, reads=(), writes=()):
        self._deps(eng, reads, writes)
        inst = fn(self.e[eng])
        if self.cnt[eng] >= EPOCH:
            self.sem[eng] = self._newsem(eng)
            self.cnt[eng] = 0
        self.cnt[eng] += 1
        self.tot[eng] += 1
        sem = self.sem[eng]
        inst.then_inc(sem, 1)
        ev = (sem, self.cnt[eng], eng)
        self._mark(ev, reads, writes, eng)
        return ev

    def dma(self, eng, out, in_, reads=(), writes=(), **kw):
        self._deps(eng, reads, writes)
        slot = self.dma_sems[self.dma_rr]
        self.dma_rr = (self.dma_rr + 1) % len(self.dma_sems)
        sem = slot[0]
        if slot[1] > 0:
            self._wait(eng, (sem, slot[1], "dma"))
        inst = self.e[eng].dma_start(out=out, in_=in_, **kw)
        slot[1] += 16
        inst.then_inc(sem, 16)
        ev = (sem, slot[1], "dma")
        self.tot[eng] += 1
        self._mark(ev, reads, writes, "dma%d" % id(sem))
        return ev

    def barrier(self, bufs):
        for eng in self.ENG:
            for b in bufs:
                self._wait(eng, b.w)
                for ev in b.r.values():
                    self._wait(eng, ev)

    def drain_dmas(self, eng="sp"):
        for sem, val in self.dma_sems:
            if val > 0:
                self._wait(eng, (sem, val, "dma"))


def build_program(n_layers=DEPTH, do_moe=True, dbg=False):
    nc = bass.Bass("TRN2", target_bir_lowering=False)
    P = 128

    def din(name, shape):
        return nc.dram_tensor(name, list(shape), F32, kind="ExternalInput").ap()

    x_in = din("x", [L, D])
    w_in = din("w_in", [n_layers, D, NCOL])
    conv_w = din("conv_w", [n_layers, 3, 512])
    w_out = din("w_out", [n_layers, D, D])
    ln1_g = din("ln1_g", [n_layers, D]); ln1_b = din("ln1_b", [n_layers, D])
    ln2_g = din("ln2_g", [n_layers, D]); ln2_b = din("ln2_b", [n_layers, D])
    if do_moe:
        router_w = din("router_w", [n_layers, D, NE]); router_b = din("router_b", [n_layers, NE])
        w_gu = din("w_gu", [n_layers, NE, D, 2 * DFF]); b_gu = din("b_gu", [n_layers, NE, 2 * DFF])
        w_down = din("w_down", [n_layers, NE, DFF, D]); b_down = din("b_down", [n_layers, NE, D])
    c_ident = din("c_ident", [P, P])
    c_trineg = din("c_trineg", [P, P])
    c_tri4 = din("c_tri4", [P, 4 * P])
    c_pow2 = din("c_pow2", [P, NBIS])
    c_rot = din("c_rot", [4, L, 256])
    y_out = nc.dram_tensor("y", [L, D], F32, kind="ExternalOutput").ap()
    x1_kind = "ExternalOutput" if dbg else "Internal"
    x1_d = nc.dram_tensor("x1_d", [L, D], F32, kind=x1_kind).ap()
    xa_d = nc.dram_tensor("xa_d", [L, D], F32, kind="Internal").ap()
    xb_d = nc.dram_tensor("xb_d", [L, D], F32, kind="Internal").ap()
    x1T_d = nc.dram_tensor("x1T_d", [D, L], BF16, kind="Internal").ap()
    macc_d = nc.dram_tensor("macc_d", [L, D], F32, kind="Internal").ap()

    gam = [1.0 - 2.0 ** (-5 - h) for h in range(4)]
    cdec = [g ** 128 for g in gam]

    with ExitStack() as es:
        S = Sched(nc, es)

        uid = [0]

        def sb(stack, name, shape, dt):
            uid[0] += 1
            return Buf(stack.enter_context(nc.sbuf_tensor(f"{name}_{uid[0]}", list(shape), dt)), name)

        pbig = [es.enter_context(nc.psum_tensor(f"pbig{i}", [P, 1024], F32)) for i in range(3)]
        psb = [Buf(pbig[i // 2][:, (i % 2) * 512:(i % 2 + 1) * 512], f"ps{i}") for i in range(6)]
        pst = [Buf(es.enter_context(nc.psum_tensor(f"pst{i}", [P, 1024], BF16)), f"pst{i}") for i in range(2)]
        rr = {"mm": 0, "acc": 0, "tr": 0}

        def ps_mm():
            rr["mm"] = (rr["mm"] + 1) % 4
            return psb[rr["mm"]]

        def ps_acc():
            rr["acc"] = (rr["acc"] + 1) % 2
            return psb[4 + rr["acc"]]

        def ps_tr():
            rr["tr"] = (rr["tr"] + 1) % 2
            return pst[rr["tr"]]

        ident = sb(es, "ident", [P, P], BF16)
        trineg = sb(es, "trineg", [P, P], F32)
        tri4 = sb(es, "tri4", [P, 4 * P], BF16)
        pow2 = sb(es, "pow2", [P, NBIS], F32)
        ones_bf = sb(es, "ones_bf", [P, P], BF16)
        S.dma("pool", ident[:], c_ident[:, :], writes=[ident])
        S.dma("pool", tri4[:], c_tri4[:, :], writes=[tri4])
        S.dma("sp", trineg[:], c_trineg[:, :], writes=[trineg])
        S.dma("sp", pow2[:], c_pow2[:, :], writes=[pow2])
        S.op("dve", lambda e: e.memset(ones_bf[:], 1.0), writes=[ones_bf])

        def dtiles(ap):
            return [Buf(ap[t * P:(t + 1) * P, :]) for t in range(NT)]
        xa_t = dtiles(xa_d); xb_t = dtiles(xb_d); x1_t = dtiles(x1_d); macc_t = dtiles(macc_d); y_t = dtiles(y_out)
        xin_t = dtiles(x_in)
        x1T_g = [Buf(x1T_d[:, g * GW:(g + 1) * GW]) for g in range(NG)]

        def layer_norm_tile(zb, z, gb, g_bc, bb, b_bc, scr):
            st, junk, jap = scr["st"], scr["junk"], scr["junk_ap"]
            S.op("dve", lambda e: e.tensor_reduce(st[:, 0:1], z, AX.X, ALU.add), reads=[zb], writes=[st])
            S.op("dve", lambda e: e.tensor_scalar(st[:, 1:2], st[:, 0:1], -1.0 / D, None, op0=ALU.mult), reads=[st], writes=[st])
            S.op("dve", lambda e: e.tensor_scalar(z, z, st[:, 1:2], None, op0=ALU.add), reads=[zb, st], writes=[zb])
            S.op("act", lambda e: e.activation(jap, z, AF.Square, accum_out=st[:, 2:3]), reads=[zb, st], writes=[junk, st])
            S.op("dve", lambda e: e.tensor_scalar(st[:, 3:4], st[:, 2:3], 1.0 / D, EPS, op0=ALU.mult, op1=ALU.add), reads=[st], writes=[st])
            S.op("act", lambda e: e.sqrt(st[:, 4:5], st[:, 3:4]), reads=[st], writes=[st])
            S.op("dve", lambda e: e.reciprocal(st[:, 4:5], st[:, 4:5]), reads=[st], writes=[st])
            S.op("dve", lambda e: e.scalar_tensor_tensor(z, z, st[:, 4:5], g_bc, op0=ALU.mult, op1=ALU.mult), reads=[zb, st, gb], writes=[zb])
            S.op("dve", lambda e: e.tensor_tensor(z, z, b_bc, ALU.add), reads=[zb, bb], writes=[zb])

        for l in range(n_layers):
            src_t = xin_t if l == 0 else (xa_t if l % 2 == 1 else xb_t)
            src_d = x_in if l == 0 else (xa_d if l % 2 == 1 else xb_d)
            last = (l == n_layers - 1)
            if do_moe:
                dst_t = y_t if last else (xa_t if l % 2 == 0 else xb_t)
                dst_d = y_out if last else (xa_d if l % 2 == 0 else xb_d)
            else:
                dst_t = None

            with ExitStack() as ms:
                kT = sb(ms, "kT", [P, 2, L], BF16)
                Vp = sb(ms, "Vp", [P, NT, 4, 65], BF16)
                kiT = sb(ms, "kiT", [P, L], BF16)
                state_f = sb(ms, "state_f", [P, 4, P], F32)
                state_b = sb(ms, "state_b", [P, 4, P], BF16)
                cw = sb(ms, "cw", [P, 4, 3], F32)
                hal = sb(ms, "hal", [P, 4, 2], F32)
                uc = sb(ms, "uc", [P, 2 + GW], F32)
                convT = sb(ms, "convT", [P, 4, GW], BF16)
                wbuf = [sb(ms, f"wbuf{i}", [P, 16, 512], BF16) for i in range(2)]
                xg = sb(ms, "xg", [P, GT, D], F32)
                xT = sb(ms, "xT", [P, 16, GW], BF16)
                qT = sb(ms, "qT", [P, 8, GW], BF16)
                qiT = sb(ms, "qiT", [P, 4, GW], BF16)
                vv = sb(ms, "vv", [P, GT, 512], BF16)
                sgt = sb(ms, "sgt", [P, GT, 512], BF16)
                iwt = sb(ms, "iwt", [P, GT, 8], F32)
                rotb = [sb(ms, "rotb0", [P, 2, 256], F32)] * 2
                rt = [sb(ms, f"rt{i}", [P, 4, 64], F32) for i in range(2)]
                qpk = sb(ms, "qpk", [P, GT, 2, 512], BF16)
                qkT = sb(ms, "qkT", [P, 2, 4, P], BF16)
                sdT = sb(ms, "sdT", [P, 4, P], BF16)
                osb = sb(ms, "osb", [P, 4, P], F32)
                cbj_ap = osb[:].rearrange("p h t -> p (h t)")
                sm = sb(ms, "sm", [P, 16], F32)
                retk = sb(ms, "retk", [P, 512], BF16)
                sc = sb(ms, "sc", [P, L], F32)
                rl = [sb(ms, f"rl{i}", [P, 512], F32) for i in range(2)]
                ccT = rl[1]
                mskT = sb(ms, "mskT", [P, NT, P], BF16)
                mblk = sb(ms, "mblk", [P, 8 * P], BF16)
                bis = sb(ms, "bis", [P, 8], F32)
                Wt = sb(ms, "Wt", [P, NBIS], F32)
                cnt = sb(ms, "cnt", [P, NBIS], F32)
                Eb = [sb(ms, f"Eb{i}", [P, 1024], BF16) for i in range(2)]
                rden = sb(ms, "rden", [P, 4], F32)
                atk = sb(ms, "atk", [P, 1024], BF16)
                lnst = sb(ms, "lnst", [P, 8], F32)
                mix_bufs = [kT, Vp, kiT, state_f, state_b, cw, hal, uc, convT, xg, xT, qT, qiT, vv, sgt,
                            iwt, qpk, qkT, sdT, osb, sm, retk, sc, mskT, mblk, bis, Wt, cnt, rden, atk, lnst] \
                    + wbuf + rt + rl + Eb + rotb[:1]
                mflat = mskT[:].rearrange("p k t -> p (k t)")
                xbf_ap = mflat[:, 0:D]
                concatT = xT

                for k in range(3):
                    S.dma("sp", cw[:, :, k], conv_w[l, k].rearrange("(cc p) -> p cc", p=P), writes=[cw], allow_slow_non_contiguous=True)
                S.op("dve", lambda e: e.memset(state_f[:], 0.0), writes=[state_f])
                S.op("dve", lambda e: e.memset(state_b[:], 0.0), writes=[state_b])
                S.op("dve", lambda e: e.memset(hal[:], 0.0), writes=[hal])
                S.op("dve", lambda e: e.memset(Vp[:], 1.0), writes=[Vp])
                ri = [0]

                wi = [0]

                def load_w(src_ap, ncols, rows_kc=16):
                    wb = wbuf[wi[0] % 2]
                    wi[0] += 1
                    S.dma("pool", wb[:, 0:rows_kc, 0:ncols], src_ap.rearrange("(kc p) n -> p kc n", p=P), writes=[wb])
                    return wb

                for tg in range(NG):
                    t0 = tg * GT
                    c0 = tg * GW
                    for i in range(GT):
                        S.dma("sp", xg[:, i, :], src_d[(t0 + i) * P:(t0 + i + 1) * P, :], reads=[src_t[t0 + i]], writes=[xg])
                    for i in range(GT):
                        S.op("act", lambda e: e.copy(xbf_ap, xg[:, i, :]), reads=[xg], writes=[mskT])
                        for half in range(2):
                            pt = ps_tr()
                            for k in range(8):
                                kc = half * 8 + k
                                S.op("pe", lambda e: e.transpose(pt[:, k * P:(k + 1) * P], xbf_ap[:, kc * P:(kc + 1) * P], ident[:]),
                                     reads=[mskT, ident], writes=[pt])
                            S.op("dve", lambda e: e.tensor_copy(
                                xT[:, half * 8:(half + 1) * 8, i * P:(i + 1) * P],
                                pt[:].rearrange("p (k t) -> p k t", k=8)), reads=[pt], writes=[xT])
                    for fg in range(7):
                        nch = min(4, NFC - fg * 4)
                        wb = load_w(w_in[l, :, fg * 512: fg * 512 + nch * 128], nch * 128)
                        for j in range(nch):
                            fc = fg * 4 + j
                            ps = ps_mm()
                            for kc in range(16):
                                S.op("pe", lambda e: e.matmul(ps[:], lhsT=wb[:, kc, j * P:(j + 1) * P], rhs=xT[:, kc, :],
                                                              start=(kc == 0), stop=(kc == 15)), reads=[wb, xT], writes=[ps])
                            if fc < 8:
                                S.op("act", lambda e: e.activation(qT[:, fc, :], ps[:], AF.Copy, scale=0.125), reads=[ps], writes=[qT])
                            elif fc < 10:
                                S.op("act", lambda e: e.copy(kT[:, fc - 8, c0:c0 + GW], ps[:]), reads=[ps], writes=[kT])
                            elif fc < 14:
                                S.op("dve", lambda e: e.tensor_copy(qiT[:, fc - 10, :], ps[:]), reads=[ps], writes=[qiT])
                            elif fc == 14:
                                S.op("act", lambda e: e.copy(kiT[:, c0:c0 + GW], ps[:]), reads=[ps], writes=[kiT])
                            else:
                                cj, kind = divmod(fc - 15, 3)
                                if kind == 0:
                                    S.op("act", lambda e: e.copy(cbj_ap, ps[:]), reads=[ps], writes=[osb])
                                elif kind == 1:
                                    S.op("act", lambda e: e.copy(uc[:, 2:2 + GW], ps[:]), reads=[ps], writes=[uc])
                                    S.op("dve", lambda e: e.tensor_copy(uc[:, 0:2], hal[:, cj, :]), reads=[hal], writes=[uc])
                                else:
                                    S.op("dve", lambda e: e.tensor_tensor(uc[:, 2:2 + GW], uc[:, 2:2 + GW], ps[:], ALU.mult), reads=[ps, uc], writes=[uc])
                                    S.op("dve", lambda e: e.tensor_copy(hal[:, cj, :], uc[:, GW:GW + 2]), reads=[uc], writes=[hal])
                                    S.op("dve", lambda e: e.tensor_scalar(ccT[:], uc[:, 2:2 + GW], cw[:, cj, 2:3], None, op0=ALU.mult),
                                         reads=[uc, cw], writes=[ccT])
                                    S.op("dve", lambda e: e.scalar_tensor_tensor(ccT[:], uc[:, 1:1 + GW], cw[:, cj, 1:2], ccT[:], op0=ALU.mult, op1=ALU.add),
                                         reads=[uc, cw, ccT], writes=[ccT])
                                    S.op("dve", lambda e: e.scalar_tensor_tensor(ccT[:], uc[:, 0:GW], cw[:, cj, 0:1], ccT[:], op0=ALU.mult, op1=ALU.add),
                                         reads=[uc, cw, ccT], writes=[ccT])
                                    S.op("dve", lambda e: e.tensor_tensor(convT[:, cj, :], ccT[:], cbj_ap, ALU.mult), reads=[ccT, osb], writes=[convT])
                    tm_groups = [(0, 512), (512, 512), (1024, 512), (1536, 512), (2048, 264)]
                    for gi, (tc0, tn) in enumerate(tm_groups):
                        wb = load_w(w_in[l, :, FCOLS + tc0: FCOLS + tc0 + tn], tn)
                        for i in range(GT):
                            p = t0 + i
                            if gi < 2:
                                rb_ = rotb[ri[0] % 2]
                                ri[0] += 1
                                S.dma("sp", rb_[:], c_rot[2 * gi:2 * gi + 2, p * P:(p + 1) * P, :].rearrange("w p c -> p w c"), writes=[rb_])
                            ps = ps_mm()
                            for kc in range(16):
                                S.op("pe", lambda e: e.matmul(ps[:, 0:tn], lhsT=xT[:, kc, i * P:(i + 1) * P], rhs=wb[:, kc, 0:tn],
                                                              start=(kc == 0), stop=(kc == 15)), reads=[wb, xT], writes=[ps])
                            if gi < 2:
                                xv = ps[:].rearrange("p (h two d) -> p h two d", two=2, d=64)
                                x1v, x2v = xv[:, :, 0, :], xv[:, :, 1, :]
                                C = rb_[:, 0, :].rearrange("p (h d) -> p h d", d=64)
                                Sn = rb_[:, 1, :].rearrange("p (h d) -> p h d", d=64)
                                ov = qpk[:, i, gi, :].rearrange("p (h two d) -> p h two d", two=2, d=64)
                                S.op("dve", lambda e: e.tensor_tensor(rt[0][:], x1v, C, ALU.mult), reads=[ps, rb_], writes=[rt[0]])
                                S.op("dve", lambda e: e.tensor_tensor(rt[1][:], x2v, Sn, ALU.mult), reads=[ps, rb_], writes=[rt[1]])
                                S.op("dve", lambda e: e.tensor_tensor(ov[:, :, 0, :], rt[0][:], rt[1][:], ALU.subtract), reads=[rt[0], rt[1]], writes=[qpk])
                                S.op("dve", lambda e: e.tensor_tensor(rt[0][:], x1v, Sn, ALU.mult), reads=[ps, rb_], writes=[rt[0]])
                                S.op("dve", lambda e: e.tensor_tensor(rt[1][:], x2v, C, ALU.mult), reads=[ps, rb_], writes=[rt[1]])
                                S.op("dve", lambda e: e.tensor_tensor(ov[:, :, 1, :], rt[0][:], rt[1][:], ALU.add), reads=[rt[0], rt[1]], writes=[qpk])
                            elif gi == 2:
                                S.op("act", lambda e: e.copy(vv[:, i, :], ps[:]), reads=[ps], writes=[vv])
                            elif gi == 3:
                                S.op("act", lambda e: e.activation(sgt[:, i, :], ps[:], AF.Silu), reads=[ps], writes=[sgt])
                            else:
                                S.op("dve", lambda e: e.tensor_copy(
                                    Vp[:, p, :, 0:64], ps[:, 0:256].rearrange("p (g d) -> p g d", g=4)), reads=[ps], writes=[Vp])
                                S.op("dve", lambda e: e.tensor_scalar(iwt[:, i, :], ps[:, 256:264], 512.0 ** -0.5, None, op0=ALU.mult),
                                     reads=[ps], writes=[iwt])
                    S.barrier([xT])
                    for cj in range(4):
                        S.op("act", lambda e: e.copy(concatT[:, 8 + cj, :], convT[:, cj, :]), reads=[convT], writes=[concatT])

                    for i in range(GT):
                        p = t0 + i
                        tcol = slice(i * P, (i + 1) * P)
                        pt = ps_tr()
                        for which in range(2):
                            for h in range(4):
                                S.op("pe", lambda e: e.transpose(pt[:, (which * 4 + h) * P:(which * 4 + h + 1) * P],
                                                                 qpk[:, i, which, h * P:(h + 1) * P], ident[:]), reads=[qpk, ident], writes=[pt])
                        S.op("act", lambda e: e.copy(qkT[:].rearrange("p a h t -> p (a h t)"), pt[:]), reads=[pt], writes=[qkT])
                        ps = ps_mm()
                        for h in range(4):
                            S.op("pe", lambda e: e.matmul(ps[:, h * P:(h + 1) * P], lhsT=qkT[:, 1, h, :], rhs=qkT[:, 0, h, :], start=True, stop=True),
                                 reads=[qkT], writes=[ps])
                        S.op("dve", lambda e: e.tensor_tensor(sdT[:].rearrange("p h t -> p (h t)"), ps[:], tri4[:], ALU.mult), reads=[ps, tri4], writes=[sdT])
                        po = ps_acc()
                        for h in range(4):
                            S.op("pe", lambda e: e.matmul(po[:, h * P:(h + 1) * P], lhsT=sdT[:, h, :], rhs=vv[:, i, h * P:(h + 1) * P], start=True, stop=False),
                                 reads=[sdT, vv], writes=[po])
                            S.op("pe", lambda e: e.matmul(po[:, h * P:(h + 1) * P], lhsT=qkT[:, 0, h, :], rhs=state_b[:, h, :], start=False, stop=True),
                                 reads=[qkT, state_b], writes=[po])
                        pa = ps_mm()
                        for h in range(4):
                            S.op("pe", lambda e: e.matmul(pa[:, h * P:(h + 1) * P], lhsT=qpk[:, i, 1, h * P:(h + 1) * P], rhs=vv[:, i, h * P:(h + 1) * P], start=True, stop=True),
                                 reads=[qpk, vv], writes=[pa])
                        S.op("dve", lambda e: e.tensor_tensor(state_f[:].rearrange("p h t -> p (h t)"), state_f[:].rearrange("p h t -> p (h t)"), pa[:], ALU.add),
                             reads=[pa, state_f], writes=[state_f])
                        for h in range(4):
                            S.op("dve", lambda e: e.tensor_scalar(state_f[:, h, :], state_f[:, h, :], float(cdec[h]), None, op0=ALU.mult),
                                 reads=[state_f], writes=[state_f])
                        S.op("act", lambda e: e.copy(state_b[:], state_f[:]), reads=[state_f], writes=[state_b])
                        osq = rl[0]
                        S.op("act", lambda e: e.copy(osb[:].rearrange("p h t -> p (h t)"), po[:]), reads=[po], writes=[osb])
                        S.op("dve", lambda e: e.tensor_reduce(sm[:, 0:4], osb[:], AX.X, ALU.add), reads=[osb], writes=[sm])
                        S.op("dve", lambda e: e.tensor_tensor(osq[:], osb[:].rearrange("p h t -> p (h t)"), osb[:].rearrange("p h t -> p (h t)"), ALU.mult), reads=[osb], writes=[osq])
                        S.op("dve", lambda e: e.tensor_reduce(sm[:, 4:8], osq[:].rearrange("p (h t) -> p h t", h=4), AX.X, ALU.add), reads=[osq], writes=[sm])
                        S.op("dve", lambda e: e.tensor_scalar(sm[:, 0:4], sm[:, 0:4], 1.0 / 128, None, op0=ALU.mult), reads=[sm], writes=[sm])
                        S.op("dve", lambda e: e.tensor_tensor(sm[:, 8:12], sm[:, 0:4], sm[:, 0:4], ALU.mult), reads=[sm], writes=[sm])
                        S.op("dve", lambda e: e.scalar_tensor_tensor(sm[:, 12:16], sm[:, 4:8], 1.0 / 128, sm[:, 8:12], op0=ALU.mult, op1=ALU.subtract),
                             reads=[sm], writes=[sm])
                        S.op("dve", lambda e: e.tensor_scalar(sm[:, 12:16], sm[:, 12:16], EPS, None, op0=ALU.add), reads=[sm], writes=[sm])
                        S.op("act", lambda e: e.sqrt(sm[:, 12:16], sm[:, 12:16]), reads=[sm], writes=[sm])
                        S.op("dve", lambda e: e.reciprocal(sm[:, 12:16], sm[:, 12:16]), reads=[sm], writes=[sm])
                        for h in range(4):
                            S.op("dve", lambda e: e.tensor_scalar(osb[:, h, :], osb[:, h, :], sm[:, h:h + 1], sm[:, 12 + h:13 + h], op0=ALU.subtract, op1=ALU.mult),
                                 reads=[osb, sm], writes=[osb])
                        S.op("dve", lambda e: e.tensor_tensor(retk[:], osb[:].rearrange("p h t -> p (h t)"), sgt[:, i, :], ALU.mult), reads=[osb, sgt], writes=[retk])
                        pt = ps_tr()
                        for h in range(4):
                            S.op("pe", lambda e: e.transpose(pt[:, h * P:(h + 1) * P], retk[:, h * P:(h + 1) * P], ident[:]), reads=[retk, ident], writes=[pt])
                        S.op("act", lambda e: e.copy(concatT[:, 12:16, tcol], pt[:, 0:512].rearrange("p (h t) -> p h t", h=4)), reads=[pt], writes=[concatT])

                        nk = (p + 1) * P
                        for cs in range(0, nk, 512):
                            cn = min(512, nk - cs)
                            for h in range(8):
                                ps = ps_mm()
                                hs = slice((h % 2) * 64, (h % 2) * 64 + 64)
                                S.op("pe", lambda e: e.matmul(ps[:, 0:cn], lhsT=qiT[hs, h // 2, tcol], rhs=kiT[hs, cs:cs + cn], start=True, stop=True),
                                     reads=[qiT, kiT], writes=[ps])
                                r = rl[h % 2]
                                S.op("act", lambda e: e.activation(r[:, 0:cn], ps[:, 0:cn], AF.Relu), reads=[ps], writes=[r])
                                if h == 0:
                                    S.op("dve", lambda e: e.tensor_scalar(sc[:, cs:cs + cn], r[:, 0:cn], iwt[:, i, 0:1], None, op0=ALU.mult),
                                         reads=[r, iwt], writes=[sc])
                                else:
                                    S.op("dve", lambda e: e.scalar_tensor_tensor(sc[:, cs:cs + cn], r[:, 0:cn], iwt[:, i, h:h + 1], sc[:, cs:cs + cn],
                                                                                 op0=ALU.mult, op1=ALU.add), reads=[r, iwt, sc], writes=[sc])
                        S.op("dve", lambda e: e.tensor_reduce(bis[:, 0:1], sc[:, 0:nk], AX.X, ALU.min), reads=[sc], writes=[bis])
                        S.op("dve", lambda e: e.tensor_reduce(bis[:, 1:2], sc[:, 0:nk], AX.X, ALU.max), reads=[sc], writes=[bis])
                        S.op("dve", lambda e: e.tensor_tensor(bis[:, 2:3], bis[:, 1:2], bis[:, 0:1], ALU.subtract), reads=[bis], writes=[bis])
                        S.op("dve", lambda e: e.tensor_scalar(Wt[:], pow2[:], bis[:, 2:3], None, op0=ALU.mult), reads=[bis, pow2], writes=[Wt])
                        S.op("dve", lambda e: e.tensor_tensor(sc[:, p * P:(p + 1) * P], sc[:, p * P:(p + 1) * P], trineg[:], ALU.add), reads=[sc, trineg], writes=[sc])
                        S.op("dve", lambda e: e.memset(cnt[:], 0.0), writes=[cnt])
                        if nk > TOPK:
                            for n in range(NBIS):
                                S.op("dve", lambda e: e.tensor_tensor(bis[:, 3:4], bis[:, 0:1], Wt[:, n:n + 1], ALU.add), reads=[bis, Wt], writes=[bis])
                                S.op("dve", lambda e: e.tensor_scalar(mflat[:, 0:nk], sc[:, 0:nk], bis[:, 3:4], 0.0, op0=ALU.is_ge, op1=ALU.add,
                                                                      accum_out=cnt[:, n:n + 1]), reads=[sc, bis, cnt], writes=[mskT, cnt])
                                S.op("dve", lambda e: e.tensor_scalar(bis[:, 4:5], cnt[:, n:n + 1], TOPK - 0.5, Wt[:, n:n + 1], op0=ALU.is_ge, op1=ALU.mult),
                                     reads=[cnt, Wt], writes=[bis])
                                S.op("dve", lambda e: e.tensor_tensor(bis[:, 0:1], bis[:, 0:1], bis[:, 4:5], ALU.add), reads=[bis], writes=[bis])
                        for b0 in range(0, p + 1, 8):
                            nb = min(8, p + 1 - b0)
                            S.op("dve", lambda e: e.tensor_scalar(mblk[:, 0:nb * P], sc[:, b0 * P:(b0 + nb) * P], bis[:, 0:1], None, op0=ALU.is_ge),
                                 reads=[sc, bis], writes=[mblk])
                            pt = ps_tr()
                            for k in range(nb):
                                S.op("pe", lambda e: e.transpose(pt[:, k * P:(k + 1) * P], mblk[:, k * P:(k + 1) * P], ident[:]),
                                     reads=[mblk, ident], writes=[pt])
                            S.op("act", lambda e: e.copy(mskT[:, b0:b0 + nb, :], pt[:, 0:nb * P].rearrange("p (k t) -> p k t", k=nb)), reads=[pt], writes=[mskT])
                        ei = 0
                        for gp in range(2):
                            accs = [psb[4], psb[5]]
                            for kb in range(p + 1):
                                bj = ei % 2
                                big_ap = pbig[bj]
                                hb = [psb[2 * bj], psb[2 * bj + 1]]
                                for hf in range(2):
                                    hs = slice(hf * 64, hf * 64 + 64)
                                    for r4 in range(4):
                                        c_ = (hf * 4 + r4) * P
                                        S.op("pe", lambda e: e.matmul(big_ap[:, c_:c_ + P], lhsT=kT[hs, gp, kb * P:(kb + 1) * P], rhs=qT[hs, gp * 4 + r4, tcol],
                                                                      start=True, stop=True), reads=[kT, qT], writes=[hb[hf]])
                                E = Eb[ei % 2]
                                ei += 1
                                S.op("act", lambda e: e.activation(E[:], big_ap[:, :], AF.Exp), reads=hb, writes=[E])
                                S.op("dve", lambda e: e.tensor_tensor(E[:].rearrange("p (r t) -> p r t", r=8), E[:].rearrange("p (r t) -> p r t", r=8),
                                                                      mskT[:, kb:kb + 1, :].to_broadcast([P, 8, P]), ALU.mult), reads=[E, mskT], writes=[E])
                                for hf in range(2):
                                    for r4 in range(4):
                                        c_ = (hf * 4 + r4) * P
                                        S.op("pe", lambda e: e.matmul(accs[hf][:, r4 * P:r4 * P + 65], lhsT=E[:, c_:c_ + P], rhs=Vp[:, kb, 2 * gp + hf, :],
                                                                      start=(kb == 0), stop=(kb == p)), reads=[E, Vp], writes=[accs[hf]])
                            for hf in range(2):
                                g = 2 * gp + hf
                                po = accs[hf]
                                pov = po[:].rearrange("p (r c) -> p r c", r=4)
                                S.op("dve", lambda e: e.reciprocal(rden[:], pov[:, :, 64]), reads=[po], writes=[rden])
                                for r4 in range(4):
                                    hh = g * 4 + r4
                                    S.op("dve", lambda e: e.tensor_scalar(atk[:, hh * 64:(hh + 1) * 64], po[:, r4 * P:r4 * P + 64], rden[:, r4:r4 + 1], None, op0=ALU.mult),
                                         reads=[po, rden], writes=[atk])
                        pt = ps_tr()
                        for k in range(8):
                            S.op("pe", lambda e: e.transpose(pt[:, k * P:(k + 1) * P], atk[:, k * P:(k + 1) * P], ident[:]), reads=[atk, ident], writes=[pt])
                        S.op("act", lambda e: e.copy(concatT[:, 0:8, tcol], pt[:].rearrange("p (k t) -> p k t", k=8)), reads=[pt], writes=[concatT])

                    S.dma("sp", sc[:, 0:D], ln1_g[l:l + 1, :].to_broadcast([P, D]), writes=[sc])
                    S.dma("sp", sc[:, D:2 * D], ln1_b[l:l + 1, :].to_broadcast([P, D]), writes=[sc])
                    for cg in range(4):
                        wb = load_w(w_out[l, :, cg * 512:(cg + 1) * 512], 512)
                        for i in range(GT):
                            ps = ps_mm()
                            for kc in range(16):
                                S.op("pe", lambda e: e.matmul(ps[:], lhsT=concatT[:, kc, i * P:(i + 1) * P], rhs=wb[:, kc, :], start=(kc == 0), stop=(kc == 15)),
                                     reads=[wb, concatT], writes=[ps])
                            S.op("dve", lambda e: e.scalar_tensor_tensor(xg[:, i, cg * 512:(cg + 1) * 512], xg[:, i, cg * 512:(cg + 1) * 512], float(ALPHA), ps[:],
                                                                         op0=ALU.mult, op1=ALU.add), reads=[ps, xg], writes=[xg])
                    S.barrier([concatT])
                    for i in range(GT):
                        p = t0 + i
                        layer_norm_tile(xg, xg[:, i, :], sc, sc[:, 0:D], sc, sc[:, D:2 * D], {"st": lnst, "junk": mskT, "junk_ap": mflat[:, 0:D]})
                        S.dma("sp", x1_d[p * P:(p + 1) * P, :], xg[:, i, :], reads=[xg], writes=[x1_t[p]])
                        if do_moe:
                            S.op("act", lambda e: e.copy(xbf_ap, xg[:, i, :]), reads=[xg], writes=[mskT])
                            for half in range(2):
                                pt = ps_tr()
                                for k in range(8):
                                    kc = half * 8 + k
                                    S.op("pe", lambda e: e.transpose(pt[:, k * P:(k + 1) * P], xbf_ap[:, kc * P:(kc + 1) * P], ident[:]), reads=[mskT, ident], writes=[pt])
                                S.op("dve", lambda e: e.tensor_copy(concatT[:, half * 8:(half + 1) * 8, i * P:(i + 1) * P],
                                                                    pt[:].rearrange("p (k t) -> p k t", k=8)), reads=[pt], writes=[concatT])
                    if do_moe:
                        S.dma("sp", x1T_d[:, c0:c0 + GW].rearrange("(kc p) t -> p kc t", p=P), concatT[:], reads=[concatT], writes=[x1T_g[tg]])
                    S.barrier([concatT])
                S.barrier(mix_bufs)

            if not do_moe:
                continue
            with ExitStack() as ms:
                wgus = [sb(ms, f"wgu{i}", [P, 16, 2 * DFF], BF16) for i in range(2)]
                wd = sb(ms, "wd", [P, 6, D], BF16)
                bgu = sb(ms, "bgu", [P, NE, 6, 2], F32)
                cb16 = sb(ms, "cb16", [P, NE], BF16)
                combT = sb(ms, "combT", [NE, P], BF16)
                rw = sb(ms, "rw", [P, 16, NE], BF16)
                rb = sb(ms, "rb", [1, NE], BF16)
                xt = [sb(ms, f"xt{i}", [P, 16, GW], BF16) for i in range(2)]
                comb = sb(ms, "comb", [P, NT, NE], F32)
                lg = sb(ms, "lg", [P, NE], F32)
                mx8 = sb(ms, "mx8", [P, 8], F32)
                rs = sb(ms, "rs", [P, 4], F32)
                actT = sb(ms, "actT", [P, 6, GW], BF16)
                tgs = [sb(ms, f"tg{i}", [P, GW], F32) for i in range(2)]
                tus = [sb(ms, f"tu{i}", [P, GW], F32) for i in range(2)]
                tss = [sb(ms, f"ts{i}", [P, GW], F32) for i in range(2)]
                stg = [sb(ms, f"stg{i}", [P, D], F32) for i in range(2)]
                lnst = sb(ms, "lnst2", [P, 8], F32)
                moe_bufs = [wd, bgu, cb16, combT, rw, rb, comb, lg, mx8, rs, actT, lnst] + xt + stg + wgus + tgs + tus + tss

                S.dma("pool", rw[:], router_w[l].rearrange("(kc p) n -> p kc n", p=P), writes=[rw])
                S.dma("pool", rb[:], router_b[l:l + 1, :], writes=[rb])
                for e_ in range(NE):
                    S.dma("sp", bgu[:, e_, :, :], b_gu[l, e_].rearrange("(fc p two) -> p fc two", p=P, two=2), writes=[bgu], allow_slow_non_contiguous=True)

                xi = [0]

                def load_xt(g):
                    b = xt[xi[0] % 2]
                    xi[0] += 1
                    S.dma("sp", b[:], x1T_d[:, g * GW:(g + 1) * GW].rearrange("(kc p) t -> p kc t", p=P), reads=[x1T_g[g]], writes=[b])
                    return b

                def load_wgu(ex_):
                    wb_ = wgus[ex_ % 2]
                    for kq in range(4):
                        S.dma("pool", wb_[:, kq * 4:(kq + 1) * 4, :], w_gu[l, ex_, kq * 512:(kq + 1) * 512, :].rearrange("(kc p) n -> p kc n", p=P), writes=[wb_])

                pre = set()

                def acc_load(tt_):
                    sb_ = stg[tt_ % 2]
                    S.dma("sp", sb_[:], macc_d[tt_ * P:(tt_ + 1) * P, :], reads=[macc_t[tt_]], writes=[sb_])

                load_wgu(0)
                nxt = load_xt(0)
                fi = 0
                for ex in range(NE):
                    wgu = wgus[ex % 2]
                    if ex + 1 < NE:
                        load_wgu(ex + 1)
                    S.dma("pool", wd[:], w_down[l, ex].rearrange("(fc p) n -> p fc n", p=P), writes=[wd])
                    for g in range(NG):
                        xb_ = nxt
                        if not (ex == NE - 1 and g == NG - 1):
                            nxt = load_xt((g + 1) % NG)
                        if ex == 0:
                            for i in range(GT):
                                tt = g * GT + i
                                ps = ps_mm()
                                for kc in range(16):
                                    S.op("pe", lambda e: e.matmul(ps[:, 0:NE], lhsT=xb_[:, kc, i * P:(i + 1) * P], rhs=rw[:, kc, :], start=(kc == 0), stop=False),
                                         reads=[xb_, rw], writes=[ps])
                                S.op("pe", lambda e: e.matmul(ps[:, 0:NE], lhsT=ones_bf[0:1, :], rhs=rb[0:1, :], start=False, stop=True), reads=[ones_bf, rb], writes=[ps])
                                S.op("act", lambda e: e.copy(lg[:], ps[:, 0:NE]), reads=[ps], writes=[lg])
                                S.op("dve", lambda e: e.max(out=mx8[:], in_=lg[:]), reads=[lg], writes=[mx8])
                                S.op("dve", lambda e: e.tensor_scalar(rs[:, 0:1], mx8[:, 0:1], -1.0, None, op0=ALU.mult), reads=[mx8], writes=[rs])
                                S.op("act", lambda e: e.activation(comb[:, tt, :], lg[:], AF.Exp, bias=rs[:, 0:1], scale=1.0), reads=[lg, rs], writes=[comb])
                                S.op("dve", lambda e: e.tensor_scalar(lg[:], lg[:], mx8[:, 3:4], None, op0=ALU.is_ge), reads=[lg, mx8], writes=[lg])
                                S.op("dve", lambda e: e.tensor_tensor(comb[:, tt, :], comb[:, tt, :], lg[:], ALU.mult), reads=[comb, lg], writes=[comb])
                                S.op("dve", lambda e: e.tensor_reduce(rs[:, 1:2], comb[:, tt, :], AX.X, ALU.add), reads=[comb], writes=[rs])
                                S.op("dve", lambda e: e.reciprocal(rs[:, 2:3], rs[:, 1:2]), reads=[rs], writes=[rs])
                                S.op("dve", lambda e: e.tensor_scalar(comb[:, tt, :], comb[:, tt, :], rs[:, 2:3], None, op0=ALU.mult), reads=[comb, rs], writes=[comb])
                        for fc in range(6):
                            tg_, tu_, ts_ = tgs[fi % 2], tus[fi % 2], tss[fi % 2]
                            fi += 1
                            pg_ = ps_mm()
                            for kc in range(16):
                                S.op("pe", lambda e: e.matmul(pg_[:], lhsT=wgu[:, kc, fc * 256: fc * 256 + 256: 2], rhs=xb_[:, kc, :], start=(kc == 0), stop=(kc == 15)),
                                     reads=[wgu, xb_], writes=[pg_])
                            pu_ = ps_mm()
                            for kc in range(16):
                                S.op("pe", lambda e: e.matmul(pu_[:], lhsT=wgu[:, kc, fc * 256 + 1: fc * 256 + 256: 2], rhs=xb_[:, kc, :], start=(kc == 0), stop=(kc == 15)),
                                     reads=[wgu, xb_], writes=[pu_])
                            S.op("dve", lambda e: e.tensor_scalar(tg_[:], pg_[:], bgu[:, ex, fc, 0:1], 7.0, op0=ALU.add, op1=ALU.min), reads=[pg_, bgu], writes=[tg_])
                            S.op("act", lambda e: e.activation(ts_[:], tg_[:], AF.Sigmoid, scale=1.702), reads=[tg_], writes=[ts_])
                            S.op("dve", lambda e: e.tensor_scalar(tu_[:], pu_[:], bgu[:, ex, fc, 1:2], 7.0, op0=ALU.add, op1=ALU.min), reads=[pu_, bgu], writes=[tu_])
                            S.op("dve", lambda e: e.tensor_scalar(tu_[:], tu_[:], -7.0, 1.0, op0=ALU.max, op1=ALU.add), reads=[tu_], writes=[tu_])
                            S.op("dve", lambda e: e.tensor_tensor(tg_[:], tg_[:], ts_[:], ALU.mult), reads=[tg_, ts_], writes=[tg_])
                            S.op("dve", lambda e: e.tensor_tensor(actT[:, fc, :], tg_[:], tu_[:], ALU.mult), reads=[tg_, tu_], writes=[actT])
                        for i in range(GT):
                            tt = g * GT + i
                            st_ = stg[tt % 2]
                            if ex > 0 and (ex, tt) not in pre:
                                acc_load(tt)
                            nx = (ex, tt + 1) if tt + 1 < NT else (ex + 1, 0)
                            if 0 < nx[0] < NE:
                                acc_load(nx[1])
                                pre.add(nx)
                            for nq in range(4):
                                po = ps_acc()
                                for fc in range(6):
                                    S.op("pe", lambda e: e.matmul(po[:], lhsT=actT[:, fc, i * P:(i + 1) * P], rhs=wd[:, fc, nq * 512:(nq + 1) * 512], start=(fc == 0), stop=(fc == 5)),
                                         reads=[actT, wd], writes=[po])
                                if ex == 0:
                                    S.op("act", lambda e: e.activation(st_[:, nq * 512:(nq + 1) * 512], po[:], AF.Copy, scale=comb[:, tt, ex:ex + 1]),
                                         reads=[po, comb], writes=[st_])
                                else:
                                    S.op("dve", lambda e: e.scalar_tensor_tensor(st_[:, nq * 512:(nq + 1) * 512], po[:], comb[:, tt, ex:ex + 1], st_[:, nq * 512:(nq + 1) * 512],
                                                                                 op0=ALU.mult, op1=ALU.add), reads=[po, comb, st_], writes=[st_])
                            S.dma("sp", macc_d[tt * P:(tt + 1) * P, :], st_[:], reads=[st_], writes=[macc_t[tt]])
                g2, b2, lnj = wgus[0], wgus[0], wgus[1]
                g2f = wgus[0][:].rearrange("p a b -> p (a b)").bitcast(F32)
                S.dma("sp", g2f[:, 0:D], ln2_g[l:l + 1, :].to_broadcast([P, D]), writes=[g2])
                S.dma("sp", g2f[:, D:2 * D], ln2_b[l:l + 1, :].to_broadcast([P, D]), writes=[b2])
                lnj_ap = wgus[1][:].rearrange("p a b -> p (a b)")[:, 0:D]
                S.dma("pool", wd[0:NE, 0, :], b_down[l, :, :], writes=[wd])
                for tt in range(NT):
                    a_, b_ = stg[0], stg[1]
                    S.dma("sp", a_[:], x1_d[tt * P:(tt + 1) * P, :], reads=[x1_t[tt]], writes=[a_])
                    S.dma("sp", b_[:], macc_d[tt * P:(tt + 1) * P, :], reads=[macc_t[tt]], writes=[b_])
                    S.op("dve", lambda e: e.scalar_tensor_tensor(a_[:], a_[:], float(ALPHA), b_[:], op0=ALU.mult, op1=ALU.add), reads=[a_, b_], writes=[a_])
                    S.op("act", lambda e: e.copy(cb16[:], comb[:, tt, :]), reads=[comb], writes=[cb16])
                    pc = ps_mm()
                    S.op("pe", lambda e: e.matmul(pc[0:NE, 0:P], lhsT=cb16[:, :], rhs=ident[:], start=True, stop=True), reads=[cb16, ident], writes=[pc])
                    S.op("act", lambda e: e.copy(combT[:], pc[0:NE, 0:P]), reads=[pc], writes=[combT])
                    for nq in range(4):
                        po = ps_acc()
                        S.op("pe", lambda e: e.matmul(po[:], lhsT=combT[:, :], rhs=wd[0:NE, 0, nq * 512:(nq + 1) * 512], start=True, stop=True),
                             reads=[combT, wd], writes=[po])
                        S.op("dve", lambda e: e.tensor_tensor(a_[:, nq * 512:(nq + 1) * 512], a_[:, nq * 512:(nq + 1) * 512], po[:], ALU.add),
                             reads=[po, a_], writes=[a_])
                    layer_norm_tile(a_, a_[:], g2, g2f[:, 0:D], b2, g2f[:, D:2 * D], {"st": lnst, "junk": lnj, "junk_ap": lnj_ap})
                    S.dma("sp", dst_d[tt * P:(tt + 1) * P, :], a_[:], reads=[a_], writes=[dst_t[tt]])
                S.barrier(moe_bufs)

        if not do_moe:
            with ExitStack() as ms:
                tmp = sb(ms, "cp", [P, D], F32)
                for tt in range(NT):
                    S.dma("sp", tmp[:], x1_d[tt * P:(tt + 1) * P, :], reads=[x1_t[tt]], writes=[tmp])
                    S.dma("sp", y_out[tt * P:(tt + 1) * P, :], tmp[:], reads=[tmp], writes=[y_t[tt]])
                S.barrier([tmp])
        S.drain_dmas("sp")
        print("instr counts", S.tot, "sems", S.nsem)
    return nc


def _col_perm():
    aq, ak, av, iq, ik, iw = 0, 1024, 1280, 1536, 2048, 2112
    cb, cc, ch, rq, rk, rv, rg = 2120, 2632, 3144, 3656, 4168, 4680, 5192
    idx = []
    for pg in range(2):
        for r in range(4):
            for g in (2 * pg, 2 * pg + 1):
                h = 4 * g + r
                idx += list(range(aq + h * 64, aq + h * 64 + 64))
    idx += list(range(ak, ak + 256))
    idx += list(range(iq, iq + 512))
    idx += list(range(ik, ik + 64)) * 2
    for j in range(4):
        idx += list(range(cb + j * 128, cb + (j + 1) * 128)) + list(range(cc + j * 128, cc + (j + 1) * 128)) \
            + list(range(ch + j * 128, ch + (j + 1) * 128))
    idx += list(range(rq, rq + 512)) + list(range(rk, rk + 512)) + list(range(rv, rv + 512)) + list(range(rg, rg + 512))
    idx += list(range(av, av + 256)) + list(range(iw, iw + 8))
    assert len(idx) == NCOL
    return np.asarray(idx)


def _consts():
    P = 128
    t = np.arange(P)
    trineg = np.where(t[None, :] <= t[:, None], 0.0, -1e30).astype(np.float32)
    triT = (t[None, :] >= t[:, None]).astype(np.float32)
    tri4 = np.tile(triT, (1, 4))
    pow2 = np.tile((2.0 ** -(np.arange(NBIS) + 1.0))[None, :], (P, 1)).astype(np.float32)
    half = 64
    inv_freq = (10000.0 ** (-np.linspace(0.0, 1.0, half, dtype=np.float32))).astype(np.float32)
    pos = np.arange(L, dtype=np.float32)
    ang = (pos[:, None] * inv_freq[None, :]).astype(np.float32).astype(np.float64)
    cos, sin = np.cos(ang), np.sin(ang)
    i = (np.arange(L) % 128).astype(np.float64)
    rot = np.zeros((4, L, 4, 64), np.float64)
    for h in range(4):
        lgm = np.log1p(-(2.0 ** (-5.0 - h)))
        qd = np.exp((i + 1.0) * lgm)[:, None]
        kd = (128.0 ** -0.5) * np.exp(-(i + 1.0) * lgm)[:, None]
        rot[0, :, h] = cos * qd; rot[1, :, h] = sin * qd
        rot[2, :, h] = cos * kd; rot[3, :, h] = sin * kd
    return {"c_ident": np.eye(P, dtype=np.float32), "c_trineg": trineg, "c_tri4": tri4.astype(np.float32),
            "c_pow2": pow2, "c_rot": rot.reshape(4, L, 256).astype(np.float32)}


_CACHE = {}


def _run(inputs, n_layers=DEPTH, do_moe=True, dbg=False, n_cores=2):
    key = (n_layers, do_moe, dbg)
    if key not in _CACHE:
        _CACHE[key] = build_program(n_layers, do_moe, dbg)
    nc = _CACHE[key]
    perm = _col_perm()
    f = lambda a: np.ascontiguousarray(np.asarray(a, dtype=np.float32))
    shared = {
        "w_in": f(np.asarray(inputs["w_in"])[:n_layers][:, :, perm]),
        "conv_w": f(inputs["conv_w"][:n_layers]), "w_out": f(inputs["w_out"][:n_layers]),
        "ln1_g": f(inputs["ln1_g"][:n_layers]), "ln1_b": f(inputs["ln1_b"][:n_layers]),
        "ln2_g": f(inputs["ln2_g"][:n_layers]), "ln2_b": f(inputs["ln2_b"][:n_layers]),
    }
    if do_moe:
        for k in ("router_w", "router_b", "w_gu", "b_gu", "w_down", "b_down"):
            shared[k] = f(inputs[k][:n_layers])
    shared.update(_consts())
    x = np.asarray(inputs["x"], dtype=np.float32)
    in_maps = [dict(shared, x=np.ascontiguousarray(x[b])) for b in range(n_cores)]
    res = run_bass_kernel_spmd(nc, in_maps, core_ids=list(range(n_cores)))
    return res


def kernel(**inputs):
    res = _run(inputs)
    return np.stack([res.results[b]["y"] for b in range(2)], axis=0).astype(np.float32)
```

```python
import numpy as np
from contextlib import ExitStack
import concourse.bass as bass
import concourse.mybir as mybir
from concourse.bass_utils import run_bass_kernel_spmd

F32 = mybir.dt.float32
BF16 = mybir.dt.bfloat16
ALU = mybir.AluOpType
AF = mybir.ActivationFunctionType
AX = mybir.AxisListType

D = 2048
L = 4096
NT = 32
DEPTH = 4
NE = 32
DFF = 768
ALPHA = (2 * DEPTH) ** 0.25
EPS = 1e-5
TOPK = 256
NBIS = 14
GT = 4
NG = NT // GT
GW = GT * 128
NFC = 27
FCOLS = NFC * 128
TCOLS = 2312
NCOL = FCOLS + TCOLS
EPOCH = 30000


class Buf:
    __slots__ = ("ap", "w", "r", "name")

    def __init__(self, ap, name=""):
        self.ap = ap
        self.w = None
        self.r = {}
        self.name = name

    def __getitem__(self, idx):
        return self.ap[idx]


class Sched:
    ENG = ("pe", "act", "dve", "pool", "sp")

    def __init__(self, nc, es, n_dma_sems=32):
        self.nc = nc
        self.es = es
        self.e = {"pe": nc.tensor, "act": nc.scalar, "dve": nc.vector, "pool": nc.gpsimd, "sp": nc.sync}
        self.sem = {}
        self.cnt = {}
        self.tot = {k: 0 for k in self.ENG}
        self.nsem = 0
        for k in self.ENG:
            self.sem[k] = self._newsem(k)
            self.cnt[k] = 0
        self.dma_sems = [[self._newsem("dma"), 0] for _ in range(n_dma_sems)]
        self.dma_rr = 0
        self.seen = {k: {} for k in self.ENG}

    def _newsem(self, name):
        self.nsem += 1
        return self.es.enter_context(self.nc.semaphore(f"{name}_{self.nsem}"))

    def _wait(self, eng, ev):
        if ev is None:
            return
        sem, val, src = ev
        if src == "pe" and eng == "pe":
            return
        s = self.seen[eng]
        if s.get(id(sem), 0) >= val:
            return
        self.e[eng].wait_ge(sem, val)
        s[id(sem)] = val

    def _deps(self, eng, reads, writes):
        for b in reads:
            self._wait(eng, b.w)
        for b in writes:
            self._wait(eng, b.w)
            for ev in b.r.values():
                self._wait(eng, ev)

    def _mark(self, ev, reads, writes, key):
        for b in reads:
            b.r[key] = ev
        for b in writes:
            b.w = ev
            b.r = {}

    def op(self, eng, fn, reads=(), writes=()):
        self._deps(eng, reads, writes)
        inst = fn(self.e[eng])
        if self.cnt[eng] >= EPOCH:
            self.sem[eng] = self._newsem(eng)
            self.cnt[eng] = 0
        self.cnt[eng] += 1
        self.tot[eng] += 1
        sem = self.sem[eng]
        inst.then_inc(sem, 1)
        ev = (sem, self.cnt[eng], eng)
        self._mark(ev, reads, writes, eng)
        return ev

    def dma(self, eng, out, in_, reads=(), writes=(), **kw):
        self._deps(eng, reads, writes)
        slot = self.dma_sems[self.dma_rr]
        self.dma_rr = (self.dma_rr + 1) % len(self.dma_sems)
        sem = slot[0]
        if slot[1] > 0:
            self._wait(eng, (sem, slot[1], "dma"))
        inst = self.e[eng].dma_start(out=out, in_=in_, **kw)
        slot[1] += 16
        inst.then_inc(sem, 16)
        ev = (sem, slot[1], "dma")
        self.tot[eng] += 1
        self._mark(ev, reads, writes, "dma%d" % id(sem))
        return ev

    def barrier(self, bufs):
        for eng in self.ENG:
            for b in bufs:
                self._wait(eng, b.w)
                for ev in b.r.values():
                    self._wait(eng, ev)

    def drain_dmas(self, eng="sp"):
        for sem, val in self.dma_sems:
            if val > 0:
                self._wait(eng, (sem, val, "dma"))


def build_program(n_layers=DEPTH, do_moe=True, dbg=False):
    nc = bass.Bass("TRN2", target_bir_lowering=False)
    P = 128

    def din(name, shape):
        return nc.dram_tensor(name, list(shape), F32, kind="ExternalInput").ap()

    x_in = din("x", [L, D])
    w_in = din("w_in", [n_layers, D, NCOL])
    conv_w = din("conv_w", [n_layers, 3, 512])
    w_out = din("w_out", [n_layers, D, D])
    ln1_g = din("ln1_g", [n_layers, D]); ln1_b = din("ln1_b", [n_layers, D])
    ln2_g = din("ln2_g", [n_layers, D]); ln2_b = din("ln2_b", [n_layers, D])
    if do_moe:
        router_w = din("router_w", [n_layers, D, NE]); router_b = din("router_b", [n_layers, NE])
        w_gu = din("w_gu", [n_layers, NE, D, 2 * DFF]); b_gu = din("b_gu", [n_layers, NE, 2 * DFF])
        w_down = din("w_down", [n_layers, NE, DFF, D]); b_down = din("b_down", [n_layers, NE, D])
    c_ident = din("c_ident", [P, P])
    c_trineg = din("c_trineg", [P, P])
    c_tri4 = din("c_tri4", [P, 4 * P])
    c_pow2 = din("c_pow2", [P, NBIS])
    c_rot = din("c_rot", [4, L, 256])
    y_out = nc.dram_tensor("y", [L, D], F32, kind="ExternalOutput").ap()
    x1_kind = "ExternalOutput" if dbg else "Internal"
    x1_d = nc.dram_tensor("x1_d", [L, D], F32, kind=x1_kind).ap()
    xa_d = nc.dram_tensor("xa_d", [L, D], F32, kind="Internal").ap()
    xb_d = nc.dram_tensor("xb_d", [L, D], F32, kind="Internal").ap()
    x1T_d = nc.dram_tensor("x1T_d", [D, L], BF16, kind="Internal").ap()
    macc_d = nc.dram_tensor("macc_d", [L, D], F32, kind="Internal").ap()

    gam = [1.0 - 2.0 ** (-5 - h) for h in range(4)]
    cdec = [g ** 128 for g in gam]

    with ExitStack() as es:
        S = Sched(nc, es)

        uid = [0]

        def sb(stack, name, shape, dt):
            uid[0] += 1
            return Buf(stack.enter_context(nc.sbuf_tensor(f"{name}_{uid[0]}", list(shape), dt)), name)

        pbig = [es.enter_context(nc.psum_tensor(f"pbig{i}", [P, 1024], F32)) for i in range(3)]
        psb = [Buf(pbig[i // 2][:, (i % 2) * 512:(i % 2 + 1) * 512], f"ps{i}") for i in range(6)]
        pst = [Buf(es.enter_context(nc.psum_tensor(f"pst{i}", [P, 1024], BF16)), f"pst{i}") for i in range(2)]
        rr = {"mm": 0, "acc": 0, "tr": 0}

        def ps_mm():
            rr["mm"] = (rr["mm"] + 1) % 4
            return psb[rr["mm"]]

        def ps_acc():
            rr["acc"] = (rr["acc"] + 1) % 2
            return psb[4 + rr["acc"]]

        def ps_tr():
            rr["tr"] = (rr["tr"] + 1) % 2
            return pst[rr["tr"]]

        ident = sb(es, "ident", [P, P], BF16)
        trineg = sb(es, "trineg", [P, P], F32)
        tri4 = sb(es, "tri4", [P, 4 * P], BF16)
        pow2 = sb(es, "pow2", [P, NBIS], F32)
        ones_bf = sb(es, "ones_bf", [P, P], BF16)
        S.dma("pool", ident[:], c_ident[:, :], writes=[ident])
        S.dma("pool", tri4[:], c_tri4[:, :], writes=[tri4])
        S.dma("sp", trineg[:], c_trineg[:, :], writes=[trineg])
        S.dma("sp", pow2[:], c_pow2[:, :], writes=[pow2])
        S.op("dve", lambda e: e.memset(ones_bf[:], 1.0), writes=[ones_bf])

        def dtiles(ap):
            return [Buf(ap[t * P:(t + 1) * P, :]) for t in range(NT)]
        xa_t = dtiles(xa_d); xb_t = dtiles(xb_d); x1_t = dtiles(x1_d); macc_t = dtiles(macc_d); y_t = dtiles(y_out)
        xin_t = dtiles(x_in)
        x1T_g = [Buf(x1T_d[:, g * GW:(g + 1) * GW]) for g in range(NG)]

        def layer_norm_tile(zb, z, gb, g_bc, bb, b_bc, scr):
            st, junk, jap = scr["st"], scr["junk"], scr["junk_ap"]
            S.op("dve", lambda e: e.tensor_reduce(st[:, 0:1], z, AX.X, ALU.add), reads=[zb], writes=[st])
            S.op("dve", lambda e: e.tensor_scalar(st[:, 1:2], st[:, 0:1], -1.0 / D, None, op0=ALU.mult), reads=[st], writes=[st])
            S.op("dve", lambda e: e.tensor_scalar(z, z, st[:, 1:2], None, op0=ALU.add), reads=[zb, st], writes=[zb])
            S.op("act", lambda e: e.activation(jap, z, AF.Square, accum_out=st[:, 2:3]), reads=[zb, st], writes=[junk, st])
            S.op("dve", lambda e: e.tensor_scalar(st[:, 3:4], st[:, 2:3], 1.0 / D, EPS, op0=ALU.mult, op1=ALU.add), reads=[st], writes=[st])
            S.op("act", lambda e: e.sqrt(st[:, 4:5], st[:, 3:4]), reads=[st], writes=[st])
            S.op("dve", lambda e: e.reciprocal(st[:, 4:5], st[:, 4:5]), reads=[st], writes=[st])
            S.op("dve", lambda e: e.scalar_tensor_tensor(z, z, st[:, 4:5], g_bc, op0=ALU.mult, op1=ALU.mult), reads=[zb, st, gb], writes=[zb])
            S.op("dve", lambda e: e.tensor_tensor(z, z, b_bc, ALU.add), reads=[zb, bb], writes=[zb])

        for l in range(n_layers):
            src_t = xin_t if l == 0 else (xa_t if l % 2 == 1 else xb_t)
            src_d = x_in if l == 0 else (xa_d if l % 2 == 1 else xb_d)
            last = (l == n_layers - 1)
            if do_moe:
                dst_t = y_t if last else (xa_t if l % 2 == 0 else xb_t)
                dst_d = y_out if last else (xa_d if l % 2 == 0 else xb_d)
            else:
                dst_t = None

            with ExitStack() as ms:
                kT = sb(ms, "kT", [P, 2, L], BF16)
                Vp = sb(ms, "Vp", [P, NT, 4, 65], BF16)
                kiT = sb(ms, "kiT", [P, L], BF16)
                state_f = sb(ms, "state_f", [P, 4, P], F32)
                state_b = sb(ms, "state_b", [P, 4, P], BF16)
                cw = sb(ms, "cw", [P, 4, 3], F32)
                hal = sb(ms, "hal", [P, 4, 2], F32)
                uc = sb(ms, "uc", [P, 2 + GW], F32)
                convT = sb(ms, "convT", [P, 4, GW], BF16)
                wbuf = [sb(ms, f"wbuf{i}", [P, 16, 512], BF16) for i in range(2)]
                xg = sb(ms, "xg", [P, GT, D], F32)
                xT = sb(ms, "xT", [P, 16, GW], BF16)
                qT = sb(ms, "qT", [P, 8, GW], BF16)
                qiT = sb(ms, "qiT", [P, 4, GW], BF16)
                vv = sb(ms, "vv", [P, GT, 512], BF16)
                sgt = sb(ms, "sgt", [P, GT, 512], BF16)
                iwt = sb(ms, "iwt", [P, GT, 8], F32)
                rotb = [sb(ms, "rotb0", [P, 2, 256], F32)] * 2
                rt = [sb(ms, f"rt{i}", [P, 4, 64], F32) for i in range(2)]
                qpk = sb(ms, "qpk", [P, GT, 2, 512], BF16)
                qkT = sb(ms, "qkT", [P, 2, 4, P], BF16)
                sdT = sb(ms, "sdT", [P, 4, P], BF16)
                osb = sb(ms, "osb", [P, 4, P], F32)
                cbj_ap = osb[:].rearrange("p h t -> p (h t)")
                sm = sb(ms, "sm", [P, 16], F32)
                retk = sb(ms, "retk", [P, 512], BF16)
                sc = sb(ms, "sc", [P, L], F32)
                rl = [sb(ms, f"rl{i}", [P, 512], F32) for i in range(2)]
                ccT = rl[1]
                mskT = sb(ms, "mskT", [P, NT, P], BF16)
                mblk = sb(ms, "mblk", [P, 8 * P], BF16)
                bis = sb(ms, "bis", [P, 8], F32)
                Wt = sb(ms, "Wt", [P, NBIS], F32)
                cnt = sb(ms, "cnt", [P, NBIS], F32)
                Eb = [sb(ms, f"Eb{i}", [P, 1024], BF16) for i in range(2)]
                rden = sb(ms, "rden", [P, 4], F32)
                atk = sb(ms, "atk", [P, 1024], BF16)
                lnst = sb(ms, "lnst", [P, 8], F32)
                mix_bufs = [kT, Vp, kiT, state_f, state_b, cw, hal, uc, convT, xg, xT, qT, qiT, vv, sgt,
                            iwt, qpk, qkT, sdT, osb, sm, retk, sc, mskT, mblk, bis, Wt, cnt, rden, atk, lnst] \
                    + wbuf + rt + rl + Eb + rotb[:1]
                mflat = mskT[:].rearrange("p k t -> p (k t)")
                xbf_ap = mflat[:, 0:D]
                concatT = xT

                for k in range(3):
                    S.dma("sp", cw[:, :, k], conv_w[l, k].rearrange("(cc p) -> p cc", p=P), writes=[cw], allow_slow_non_contiguous=True)
                S.op("dve", lambda e: e.memset(state_f[:], 0.0), writes=[state_f])
                S.op("dve", lambda e: e.memset(state_b[:], 0.0), writes=[state_b])
                S.op("dve", lambda e: e.memset(hal[:], 0.0), writes=[hal])
                S.op("dve", lambda e: e.memset(Vp[:], 1.0), writes=[Vp])
                ri = [0]

                wi = [0]

                def load_w(src_ap, ncols, rows_kc=16):
                    wb = wbuf[wi[0] % 2]
                    wi[0] += 1
                    S.dma("pool", wb[:, 0:rows_kc, 0:ncols], src_ap.rearrange("(kc p) n -> p kc n", p=P), writes=[wb])
                    return wb

                for tg in range(NG):
                    t0 = tg * GT
                    c0 = tg * GW
                    for i in range(GT):
                        S.dma("sp", xg[:, i, :], src_d[(t0 + i) * P:(t0 + i + 1) * P, :], reads=[src_t[t0 + i]], writes=[xg])
                    for i in range(GT):
                        S.op("act", lambda e: e.copy(xbf_ap, xg[:, i, :]), reads=[xg], writes=[mskT])
                        for half in range(2):
                            pt = ps_tr()
                            for k in range(8):
                                kc = half * 8 + k
                                S.op("pe", lambda e: e.transpose(pt[:, k * P:(k + 1) * P], xbf_ap[:, kc * P:(kc + 1) * P], ident[:]),
                                     reads=[mskT, ident], writes=[pt])
                            S.op("dve", lambda e: e.tensor_copy(
                                xT[:, half * 8:(half + 1) * 8, i * P:(i + 1) * P],
                                pt[:].rearrange("p (k t) -> p k t", k=8)), reads=[pt], writes=[xT])
                    for fg in range(7):
                        nch = min(4, NFC - fg * 4)
                        wb = load_w(w_in[l, :, fg * 512: fg * 512 + nch * 128], nch * 128)
                        for j in range(nch):
                            fc = fg * 4 + j
                            ps = ps_mm()
                            for kc in range(16):
                                S.op("pe", lambda e: e.matmul(ps[:], lhsT=wb[:, kc, j * P:(j + 1) * P], rhs=xT[:, kc, :],
                                                              start=(kc == 0), stop=(kc == 15)), reads=[wb, xT], writes=[ps])
                            if fc < 8:
                                S.op("act", lambda e: e.activation(qT[:, fc, :], ps[:], AF.Copy, scale=0.125), reads=[ps], writes=[qT])
                            elif fc < 10:
                                S.op("act", lambda e: e.copy(kT[:, fc - 8, c0:c0 + GW], ps[:]), reads=[ps], writes=[kT])
                            elif fc < 14:
                                S.op("dve", lambda e: e.tensor_copy(qiT[:, fc - 10, :], ps[:]), reads=[ps], writes=[qiT])
                            elif fc == 14:
                                S.op("act", lambda e: e.copy(kiT[:, c0:c0 + GW], ps[:]), reads=[ps], writes=[kiT])
                            else:
                                cj, kind = divmod(fc - 15, 3)
                                if kind == 0:
                                    S.op("act", lambda e: e.copy(cbj_ap, ps[:]), reads=[ps], writes=[osb])
                                elif kind == 1:
                                    S.op("act", lambda e: e.copy(uc[:, 2:2 + GW], ps[:]), reads=[ps], writes=[uc])
                                    S.op("dve", lambda e: e.tensor_copy(uc[:, 0:2], hal[:, cj, :]), reads=[hal], writes=[uc])
                                else:
                                    S.op("dve", lambda e: e.tensor_tensor(uc[:, 2:2 + GW], uc[:, 2:2 + GW], ps[:], ALU.mult), reads=[ps, uc], writes=[uc])
                                    S.op("dve", lambda e: e.tensor_copy(hal[:, cj, :], uc[:, GW:GW + 2]), reads=[uc], writes=[hal])
                                    S.op("dve", lambda e: e.tensor_scalar(ccT[:], uc[:, 2:2 + GW], cw[:, cj, 2:3], None, op0=ALU.mult),
                                         reads=[uc, cw], writes=[ccT])
                                    S.op("dve", lambda e: e.scalar_tensor_tensor(ccT[:], uc[:, 1:1 + GW], cw[:, cj, 1:2], ccT[:], op0=ALU.mult, op1=ALU.add),
                                         reads=[uc, cw, ccT], writes=[ccT])
                                    S.op("dve", lambda e: e.scalar_tensor_tensor(ccT[:], uc[:, 0:GW], cw[:, cj, 0:1], ccT[:], op0=ALU.mult, op1=ALU.add),
                                         reads=[uc, cw, ccT], writes=[ccT])
                                    S.op("dve", lambda e: e.tensor_tensor(convT[:, cj, :], ccT[:], cbj_ap, ALU.mult), reads=[ccT, osb], writes=[convT])
                    tm_groups = [(0, 512), (512, 512), (1024, 512), (1536, 512), (2048, 264)]
                    for gi, (tc0, tn) in enumerate(tm_groups):
                        wb = load_w(w_in[l, :, FCOLS + tc0: FCOLS + tc0 + tn], tn)
                        for i in range(GT):
                            p = t0 + i
                            if gi < 2:
                                rb_ = rotb[ri[0] % 2]
                                ri[0] += 1
                                S.dma("sp", rb_[:], c_rot[2 * gi:2 * gi + 2, p * P:(p + 1) * P, :].rearrange("w p c -> p w c"), writes=[rb_])
                            ps = ps_mm()
                            for kc in range(16):
                                S.op("pe", lambda e: e.matmul(ps[:, 0:tn], lhsT=xT[:, kc, i * P:(i + 1) * P], rhs=wb[:, kc, 0:tn],
                                                              start=(kc == 0), stop=(kc == 15)), reads=[wb, xT], writes=[ps])
                            if gi < 2:
                                xv = ps[:].rearrange("p (h two d) -> p h two d", two=2, d=64)
                                x1v, x2v = xv[:, :, 0, :], xv[:, :, 1, :]
                                C = rb_[:, 0, :].rearrange("p (h d) -> p h d", d=64)
                                Sn = rb_[:, 1, :].rearrange("p (h d) -> p h d", d=64)
                                ov = qpk[:, i, gi, :].rearrange("p (h two d) -> p h two d", two=2, d=64)
                                S.op("dve", lambda e: e.tensor_tensor(rt[0][:], x1v, C, ALU.mult), reads=[ps, rb_], writes=[rt[0]])
                                S.op("dve", lambda e: e.tensor_tensor(rt[1][:], x2v, Sn, ALU.mult), reads=[ps, rb_], writes=[rt[1]])
                                S.op("dve", lambda e: e.tensor_tensor(ov[:, :, 0, :], rt[0][:], rt[1][:], ALU.subtract), reads=[rt[0], rt[1]], writes=[qpk])
                                S.op("dve", lambda e: e.tensor_tensor(rt[0][:], x1v, Sn, ALU.mult), reads=[ps, rb_], writes=[rt[0]])
                                S.op("dve", lambda e: e.tensor_tensor(rt[1][:], x2v, C, ALU.mult), reads=[ps, rb_], writes=[rt[1]])
                                S.op("dve", lambda e: e.tensor_tensor(ov[:, :, 1, :], rt[0][:], rt[1][:], ALU.add), reads=[rt[0], rt[1]], writes=[qpk])
                            elif gi == 2:
                                S.op("act", lambda e: e.copy(vv[:, i, :], ps[:]), reads=[ps], writes=[vv])
                            elif gi == 3:
                                S.op("act", lambda e: e.activation(sgt[:, i, :], ps[:], AF.Silu), reads=[ps], writes=[sgt])
                            else:
                                S.op("dve", lambda e: e.tensor_copy(
                                    Vp[:, p, :, 0:64], ps[:, 0:256].rearrange("p (g d) -> p g d", g=4)), reads=[ps], writes=[Vp])
                                S.op("dve", lambda e: e.tensor_scalar(iwt[:, i, :], ps[:, 256:264], 512.0 ** -0.5, None, op0=ALU.mult),
                                     reads=[ps], writes=[iwt])
                    S.barrier([xT])
                    for cj in range(4):
                        S.op("act", lambda e: e.copy(concatT[:, 8 + cj, :], convT[:, cj, :]), reads=[convT], writes=[concatT])

                    for i in range(GT):
                        p = t0 + i
                        tcol = slice(i * P, (i + 1) * P)
                        pt = ps_tr()
                        for which in range(2):
                            for h in range(4):
                                S.op("pe", lambda e: e.transpose(pt[:, (which * 4 + h) * P:(which * 4 + h + 1) * P],
                                                                 qpk[:, i, which, h * P:(h + 1) * P], ident[:]), reads=[qpk, ident], writes=[pt])
                        S.op("act", lambda e: e.copy(qkT[:].rearrange("p a h t -> p (a h t)"), pt[:]), reads=[pt], writes=[qkT])
                        ps = ps_mm()
                        for h in range(4):
                            S.op("pe", lambda e: e.matmul(ps[:, h * P:(h + 1) * P], lhsT=qkT[:, 1, h, :], rhs=qkT[:, 0, h, :], start=True, stop=True),
                                 reads=[qkT], writes=[ps])
                        S.op("dve", lambda e: e.tensor_tensor(sdT[:].rearrange("p h t -> p (h t)"), ps[:], tri4[:], ALU.mult), reads=[ps, tri4], writes=[sdT])
                        po = ps_acc()
                        for h in range(4):
                            S.op("pe", lambda e: e.matmul(po[:, h * P:(h + 1) * P], lhsT=sdT[:, h, :], rhs=vv[:, i, h * P:(h + 1) * P], start=True, stop=False),
                                 reads=[sdT, vv], writes=[po])
                            S.op("pe", lambda e: e.matmul(po[:, h * P:(h + 1) * P], lhsT=qkT[:, 0, h, :], rhs=state_b[:, h, :], start=False, stop=True),
                                 reads=[qkT, state_b], writes=[po])
                        pa = ps_mm()
                        for h in range(4):
                            S.op("pe", lambda e: e.matmul(pa[:, h * P:(h + 1) * P], lhsT=qpk[:, i, 1, h * P:(h + 1) * P], rhs=vv[:, i, h * P:(h + 1) * P], start=True, stop=True),
                                 reads=[qpk, vv], writes=[pa])
                        S.op("dve", lambda e: e.tensor_tensor(state_f[:].rearrange("p h t -> p (h t)"), state_f[:].rearrange("p h t -> p (h t)"), pa[:], ALU.add),
                             reads=[pa, state_f], writes=[state_f])
                        for h in range(4):
                            S.op("dve", lambda e: e.tensor_scalar(state_f[:, h, :], state_f[:, h, :], float(cdec[h]), None, op0=ALU.mult),
                                 reads=[state_f], writes=[state_f])
                        S.op("act", lambda e: e.copy(state_b[:], state_f[:]), reads=[state_f], writes=[state_b])
                        osq = rl[0]
                        S.op("act", lambda e: e.copy(osb[:].rearrange("p h t -> p (h t)"), po[:]), reads=[po], writes=[osb])
                        S.op("dve", lambda e: e.tensor_reduce(sm[:, 0:4], osb[:], AX.X, ALU.add), reads=[osb], writes=[sm])
                        S.op("dve", lambda e: e.tensor_tensor(osq[:], osb[:].rearrange("p h t -> p (h t)"), osb[:].rearrange("p h t -> p (h t)"), ALU.mult), reads=[osb], writes=[osq])
                        S.op("dve", lambda e: e.tensor_reduce(sm[:, 4:8], osq[:].rearrange("p (h t) -> p h t", h=4), AX.X, ALU.add), reads=[osq], writes=[sm])
                        S.op("dve", lambda e: e.tensor_scalar(sm[:, 0:4], sm[:, 0:4], 1.0 / 128, None, op0=ALU.mult), reads=[sm], writes=[sm])
                        S.op("dve", lambda e: e.tensor_tensor(sm[:, 8:12], sm[:, 0:4], sm[:, 0:4], ALU.mult), reads=[sm], writes=[sm])
                        S.op("dve", lambda e: e.scalar_tensor_tensor(sm[:, 12:16], sm[:, 4:8], 1.0 / 128, sm[:, 8:12], op0=ALU.mult, op1=ALU.subtract),
                             reads=[sm], writes=[sm])
                        S.op("dve", lambda e: e.tensor_scalar(sm[:, 12:16], sm[:, 12:16], EPS, None, op0=ALU.add), reads=[sm], writes=[sm])
                        S.op("act", lambda e: e.sqrt(sm[:, 12:16], sm[:, 12:16]), reads=[sm], writes=[sm])
                        S.op("dve", lambda e: e.reciprocal(sm[:, 12:16], sm[:, 12:16]), reads=[sm], writes=[sm])
                        for h in range(4):
                            S.op("dve", lambda e: e.tensor_scalar(osb[:, h, :], osb[:, h, :], sm[:, h:h + 1], sm[:, 12 + h:13 + h], op0=ALU.subtract, op1=ALU.mult),
                                 reads=[osb, sm], writes=[osb])
                        S.op("dve", lambda e: e.tensor_tensor(retk[:], osb[:].rearrange("p h t -> p (h t)"), sgt[:, i, :], ALU.mult), reads=[osb, sgt], writes=[retk])
                        pt = ps_tr()
                        for h in range(4):
                            S.op("pe", lambda e: e.transpose(pt[:, h * P:(h + 1) * P], retk[:, h * P:(h + 1) * P], ident[:]), reads=[retk, ident], writes=[pt])
                        S.op("act", lambda e: e.copy(concatT[:, 12:16, tcol], pt[:, 0:512].rearrange("p (h t) -> p h t", h=4)), reads=[pt], writes=[concatT])

                        nk = (p + 1) * P
                        for cs in range(0, nk, 512):
                            cn = min(512, nk - cs)
                            for h in range(8):
                                ps = ps_mm()
                                hs = slice((h % 2) * 64, (h % 2) * 64 + 64)
                                S.op("pe", lambda e: e.matmul(ps[:, 0:cn], lhsT=qiT[hs, h // 2, tcol], rhs=kiT[hs, cs:cs + cn], start=True, stop=True),
                                     reads=[qiT, kiT], writes=[ps])
                                r = rl[h % 2]
                                S.op("act", lambda e: e.activation(r[:, 0:cn], ps[:, 0:cn], AF.Relu), reads=[ps], writes=[r])
                                if h == 0:
                                    S.op("dve", lambda e: e.tensor_scalar(sc[:, cs:cs + cn], r[:, 0:cn], iwt[:, i, 0:1], None, op0=ALU.mult),
                                         reads=[r, iwt], writes=[sc])
                                else:
                                    S.op("dve", lambda e: e.scalar_tensor_tensor(sc[:, cs:cs + cn], r[:, 0:cn], iwt[:, i, h:h + 1], sc[:, cs:cs + cn],
                                                                                 op0=ALU.mult, op1=ALU.add), reads=[r, iwt, sc], writes=[sc])
                        S.op("dve", lambda e: e.tensor_reduce(bis[:, 0:1], sc[:, 0:nk], AX.X, ALU.min), reads=[sc], writes=[bis])
                        S.op("dve", lambda e: e.tensor_reduce(bis[:, 1:2], sc[:, 0:nk], AX.X, ALU.max), reads=[sc], writes=[bis])
                        S.op("dve", lambda e: e.tensor_tensor(bis[:, 2:3], bis[:, 1:2], bis[:, 0:1], ALU.subtract), reads=[bis], writes=[bis])
                        S.op("dve", lambda e: e.tensor_scalar(Wt[:], pow2[:], bis[:, 2:3], None, op0=ALU.mult), reads=[bis, pow2], writes=[Wt])
                        S.op("dve", lambda e: e.tensor_tensor(sc[:, p * P:(p + 1) * P], sc[:, p * P:(p + 1) * P], trineg[:], ALU.add), reads=[sc, trineg], writes=[sc])
                        S.op("dve", lambda e: e.memset(cnt[:], 0.0), writes=[cnt])
                        if nk > TOPK:
                            for n in range(NBIS):
                                S.op("dve", lambda e: e.tensor_tensor(bis[:, 3:4], bis[:, 0:1], Wt[:, n:n + 1], ALU.add), reads=[bis, Wt], writes=[bis])
                                S.op("dve", lambda e: e.tensor_scalar(mflat[:, 0:nk], sc[:, 0:nk], bis[:, 3:4], 0.0, op0=ALU.is_ge, op1=ALU.add,
                                                                      accum_out=cnt[:, n:n + 1]), reads=[sc, bis, cnt], writes=[mskT, cnt])
                                S.op("dve", lambda e: e.tensor_scalar(bis[:, 4:5], cnt[:, n:n + 1], TOPK - 0.5, Wt[:, n:n + 1], op0=ALU.is_ge, op1=ALU.mult),
                                     reads=[cnt, Wt], writes=[bis])
                                S.op("dve", lambda e: e.tensor_tensor(bis[:, 0:1], bis[:, 0:1], bis[:, 4:5], ALU.add), reads=[bis], writes=[bis])
                        for b0 in range(0, p + 1, 8):
                            nb = min(8, p + 1 - b0)
                            S.op("dve", lambda e: e.tensor_scalar(mblk[:, 0:nb * P], sc[:, b0 * P:(b0 + nb) * P], bis[:, 0:1], None, op0=ALU.is_ge),
                                 reads=[sc, bis], writes=[mblk])
                            pt = ps_tr()
                            for k in range(nb):
                                S.op("pe", lambda e: e.transpose(pt[:, k * P:(k + 1) * P], mblk[:, k * P:(k + 1) * P], ident[:]),
                                     reads=[mblk, ident], writes=[pt])
                            S.op("act", lambda e: e.copy(mskT[:, b0:b0 + nb, :], pt[:, 0:nb * P].rearrange("p (k t) -> p k t", k=nb)), reads=[pt], writes=[mskT])
                        ei = 0
                        for gp in range(2):
                            accs = [psb[4], psb[5]]
                            for kb in range(p + 1):
                                bj = ei % 2
                                big_ap = pbig[bj]
                                hb = [psb[2 * bj], psb[2 * bj + 1]]
                                for hf in range(2):
                                    hs = slice(hf * 64, hf * 64 + 64)
                                    S.op("pe", lambda e: e.matmul(big_ap[:, hf * 512:(hf + 1) * 512], lhsT=kT[hs, gp, kb * P:(kb + 1) * P],
                                                                  rhs=qT[hs, gp * 4:gp * 4 + 4, tcol], start=True, stop=True), reads=[kT, qT], writes=[hb[hf]])
                                E = Eb[ei % 2]
                                ei += 1
                                S.op("act", lambda e: e.activation(E[:], big_ap[:, :], AF.Exp), reads=hb, writes=[E])
                                S.op("dve", lambda e: e.tensor_tensor(E[:].rearrange("p (r t) -> p r t", r=8), E[:].rearrange("p (r t) -> p r t", r=8),
                                                                      mskT[:, kb:kb + 1, :].to_broadcast([P, 8, P]), ALU.mult), reads=[E, mskT], writes=[E])
                                for hf in range(2):
                                    for r4 in range(4):
                                        c_ = (hf * 4 + r4) * P
                                        S.op("pe", lambda e: e.matmul(accs[hf][:, r4 * P:r4 * P + 65], lhsT=E[:, c_:c_ + P], rhs=Vp[:, kb, 2 * gp + hf, :],
                                                                      start=(kb == 0), stop=(kb == p)), reads=[E, Vp], writes=[accs[hf]])
                            for hf in range(2):
                                g = 2 * gp + hf
                                po = accs[hf]
                                pov = po[:].rearrange("p (r c) -> p r c", r=4)
                                S.op("dve", lambda e: e.reciprocal(rden[:], pov[:, :, 64]), reads=[po], writes=[rden])
                                for r4 in range(4):
                                    hh = g * 4 + r4
                                    S.op("dve", lambda e: e.tensor_scalar(atk[:, hh * 64:(hh + 1) * 64], po[:, r4 * P:r4 * P + 64], rden[:, r4:r4 + 1], None, op0=ALU.mult),
                                         reads=[po, rden], writes=[atk])
                        pt = ps_tr()
                        for k in range(8):
                            S.op("pe", lambda e: e.transpose(pt[:, k * P:(k + 1) * P], atk[:, k * P:(k + 1) * P], ident[:]), reads=[atk, ident], writes=[pt])
                        S.op("act", lambda e: e.copy(concatT[:, 0:8, tcol], pt[:].rearrange("p (k t) -> p k t", k=8)), reads=[pt], writes=[concatT])

                    S.dma("sp", sc[:, 0:D], ln1_g[l:l + 1, :].to_broadcast([P, D]), writes=[sc])
                    S.dma("sp", sc[:, D:2 * D], ln1_b[l:l + 1, :].to_broadcast([P, D]), writes=[sc])
                    for cg in range(4):
                        wb = load_w(w_out[l, :, cg * 512:(cg + 1) * 512], 512)
                        for i in range(GT):
                            ps = ps_mm()
                            for kc in range(16):
                                S.op("pe", lambda e: e.matmul(ps[:], lhsT=concatT[:, kc, i * P:(i + 1) * P], rhs=wb[:, kc, :], start=(kc == 0), stop=(kc == 15)),
                                     reads=[wb, concatT], writes=[ps])
                            S.op("dve", lambda e: e.scalar_tensor_tensor(xg[:, i, cg * 512:(cg + 1) * 512], xg[:, i, cg * 512:(cg + 1) * 512], float(ALPHA), ps[:],
                                                                         op0=ALU.mult, op1=ALU.add), reads=[ps, xg], writes=[xg])
                    S.barrier([concatT])
                    for i in range(GT):
                        p = t0 + i
                        layer_norm_tile(xg, xg[:, i, :], sc, sc[:, 0:D], sc, sc[:, D:2 * D], {"st": lnst, "junk": mskT, "junk_ap": mflat[:, 0:D]})
                        S.dma("sp", x1_d[p * P:(p + 1) * P, :], xg[:, i, :], reads=[xg], writes=[x1_t[p]])
                        if do_moe:
                            S.op("act", lambda e: e.copy(xbf_ap, xg[:, i, :]), reads=[xg], writes=[mskT])
                            for half in range(2):
                                pt = ps_tr()
                                for k in range(8):
                                    kc = half * 8 + k
                                    S.op("pe", lambda e: e.transpose(pt[:, k * P:(k + 1) * P], xbf_ap[:, kc * P:(kc + 1) * P], ident[:]), reads=[mskT, ident], writes=[pt])
                                S.op("dve", lambda e: e.tensor_copy(concatT[:, half * 8:(half + 1) * 8, i * P:(i + 1) * P],
                                                                    pt[:].rearrange("p (k t) -> p k t", k=8)), reads=[pt], writes=[concatT])
                    if do_moe:
                        S.dma("sp", x1T_d[:, c0:c0 + GW].rearrange("(kc p) t -> p kc t", p=P), concatT[:], reads=[concatT], writes=[x1T_g[tg]])
                    S.barrier([concatT])
                S.barrier(mix_bufs)

            if not do_moe:
                continue
            with ExitStack() as ms:
                wgus = [sb(ms, f"wgu{i}", [P, 16, 2 * DFF], BF16) for i in range(2)]
                wd = sb(ms, "wd", [P, 6, D], BF16)
                bgu = sb(ms, "bgu", [P, NE, 6, 2], F32)
                cb16 = sb(ms, "cb16", [P, NE], BF16)
                combT = sb(ms, "combT", [NE, P], BF16)
                rw = sb(ms, "rw", [P, 16, NE], BF16)
                rb = sb(ms, "rb", [1, NE], BF16)
                xt = [sb(ms, f"xt{i}", [P, 16, GW], BF16) for i in range(2)]
                comb = sb(ms, "comb", [P, NT, NE], F32)
                lg = sb(ms, "lg", [P, NE], F32)
                mx8 = sb(ms, "mx8", [P, 8], F32)
                rs = sb(ms, "rs", [P, 4], F32)
                actT = sb(ms, "actT", [P, 6, GW], BF16)
                tgs = [sb(ms, f"tg{i}", [P, GW], F32) for i in range(2)]
                tus = [sb(ms, f"tu{i}", [P, GW], F32) for i in range(2)]
                tss = [sb(ms, f"ts{i}", [P, GW], F32) for i in range(2)]
                stg = [sb(ms, f"stg{i}", [P, D], F32) for i in range(2)]
                lnst = sb(ms, "lnst2", [P, 8], F32)
                moe_bufs = [wd, bgu, cb16, combT, rw, rb, comb, lg, mx8, rs, actT, lnst] + xt + stg + wgus + tgs + tus + tss

                S.dma("pool", rw[:], router_w[l].rearrange("(kc p) n -> p kc n", p=P), writes=[rw])
                S.dma("pool", rb[:], router_b[l:l + 1, :], writes=[rb])
                for e_ in range(NE):
                    S.dma("sp", bgu[:, e_, :, :], b_gu[l, e_].rearrange("(fc p two) -> p fc two", p=P, two=2), writes=[bgu], allow_slow_non_contiguous=True)

                xi = [0]

                def load_xt(g):
                    b = xt[xi[0] % 2]
                    xi[0] += 1
                    S.dma("sp", b[:], x1T_d[:, g * GW:(g + 1) * GW].rearrange("(kc p) t -> p kc t", p=P), reads=[x1T_g[g]], writes=[b])
                    return b

                def load_wgu(ex_):
                    wb_ = wgus[ex_ % 2]
                    for kq in range(4):
                        S.dma("pool", wb_[:, kq * 4:(kq + 1) * 4, :], w_gu[l, ex_, kq * 512:(kq + 1) * 512, :].rearrange("(kc p) n -> p kc n", p=P), writes=[wb_])

                pre = set()

                def acc_load(tt_):
                    sb_ = stg[tt_ % 2]
                    S.dma("sp", sb_[:], macc_d[tt_ * P:(tt_ + 1) * P, :], reads=[macc_t[tt_]], writes=[sb_])

                load_wgu(0)
                nxt = load_xt(0)
                fi = 0
                for ex in range(NE):
                    wgu = wgus[ex % 2]
                    if ex + 1 < NE:
                        load_wgu(ex + 1)
                    S.dma("pool", wd[:], w_down[l, ex].rearrange("(fc p) n -> p fc n", p=P), writes=[wd])
                    for g in range(NG):
                        xb_ = nxt
                        if not (ex == NE - 1 and g == NG - 1):
                            nxt = load_xt((g + 1) % NG)
                        if ex == 0:
                            for i in range(GT):
                                tt = g * GT + i
                                ps = ps_mm()
                                for kc in range(16):
                                    S.op("pe", lambda e: e.matmul(ps[:, 0:NE], lhsT=xb_[:, kc, i * P:(i + 1) * P], rhs=rw[:, kc, :], start=(kc == 0), stop=False),
                                         reads=[xb_, rw], writes=[ps])
                                S.op("pe", lambda e: e.matmul(ps[:, 0:NE], lhsT=ones_bf[0:1, :], rhs=rb[0:1, :], start=False, stop=True), reads=[ones_bf, rb], writes=[ps])
                                S.op("act", lambda e: e.copy(lg[:], ps[:, 0:NE]), reads=[ps], writes=[lg])
                                S.op("dve", lambda e: e.max(out=mx8[:], in_=lg[:]), reads=[lg], writes=[mx8])
                                S.op("dve", lambda e: e.tensor_scalar(rs[:, 0:1], mx8[:, 0:1], -1.0, None, op0=ALU.mult), reads=[mx8], writes=[rs])
                                S.op("act", lambda e: e.activation(comb[:, tt, :], lg[:], AF.Exp, bias=rs[:, 0:1], scale=1.0), reads=[lg, rs], writes=[comb])
                                S.op("dve", lambda e: e.tensor_scalar(lg[:], lg[:], mx8[:, 3:4], None, op0=ALU.is_ge), reads=[lg, mx8], writes=[lg])
                                S.op("dve", lambda e: e.tensor_tensor(comb[:, tt, :], comb[:, tt, :], lg[:], ALU.mult), reads=[comb, lg], writes=[comb])
                                S.op("dve", lambda e: e.tensor_reduce(rs[:, 1:2], comb[:, tt, :], AX.X, ALU.add), reads=[comb], writes=[rs])
                                S.op("dve", lambda e: e.reciprocal(rs[:, 2:3], rs[:, 1:2]), reads=[rs], writes=[rs])
                                S.op("dve", lambda e: e.tensor_scalar(comb[:, tt, :], comb[:, tt, :], rs[:, 2:3], None, op0=ALU.mult), reads=[comb, rs], writes=[comb])
                        for fc in range(6):
                            tg_, tu_, ts_ = tgs[fi % 2], tus[fi % 2], tss[fi % 2]
                            fi += 1
                            pg_ = ps_mm()
                            for kc in range(16):
                                S.op("pe", lambda e: e.matmul(pg_[:], lhsT=wgu[:, kc, fc * 256: fc * 256 + 256: 2], rhs=xb_[:, kc, :], start=(kc == 0), stop=(kc == 15)),
                                     reads=[wgu, xb_], writes=[pg_])
                            pu_ = ps_mm()
                            for kc in range(16):
                                S.op("pe", lambda e: e.matmul(pu_[:], lhsT=wgu[:, kc, fc * 256 + 1: fc * 256 + 256: 2], rhs=xb_[:, kc, :], start=(kc == 0), stop=(kc == 15)),
                                     reads=[wgu, xb_], writes=[pu_])
                            S.op("dve", lambda e: e.tensor_scalar(tg_[:], pg_[:], bgu[:, ex, fc, 0:1], 7.0, op0=ALU.add, op1=ALU.min), reads=[pg_, bgu], writes=[tg_])
                            S.op("act", lambda e: e.activation(ts_[:], tg_[:], AF.Sigmoid, scale=1.702), reads=[tg_], writes=[ts_])
                            S.op("dve", lambda e: e.tensor_scalar(tu_[:], pu_[:], bgu[:, ex, fc, 1:2], 7.0, op0=ALU.add, op1=ALU.min), reads=[pu_, bgu], writes=[tu_])
                            S.op("dve", lambda e: e.tensor_scalar(tu_[:], tu_[:], -7.0, 1.0, op0=ALU.max, op1=ALU.add), reads=[tu_], writes=[tu_])
                            S.op("dve", lambda e: e.tensor_tensor(tg_[:], tg_[:], ts_[:], ALU.mult), reads=[tg_, ts_], writes=[tg_])
                            S.op("dve", lambda e: e.tensor_tensor(actT[:, fc, :], tg_[:], tu_[:], ALU.mult), reads=[tg_, tu_], writes=[actT])
                        for i in range(GT):
                            tt = g * GT + i
                            st_ = stg[tt % 2]
                            if ex > 0 and (ex, tt) not in pre:
                                acc_load(tt)
                            nx = (ex, tt + 1) if tt + 1 < NT else (ex + 1, 0)
                            if 0 < nx[0] < NE:
                                acc_load(nx[1])
                                pre.add(nx)
                            for nq in range(4):
                                po = ps_acc()
                                for fc in range(6):
                                    S.op("pe", lambda e: e.matmul(po[:], lhsT=actT[:, fc, i * P:(i + 1) * P], rhs=wd[:, fc, nq * 512:(nq + 1) * 512], start=(fc == 0), stop=(fc == 5)),
                                         reads=[actT, wd], writes=[po])
                                if ex == 0:
                                    S.op("act", lambda e: e.activation(st_[:, nq * 512:(nq + 1) * 512], po[:], AF.Copy, scale=comb[:, tt, ex:ex + 1]),
                                         reads=[po, comb], writes=[st_])
                                else:
                                    S.op("dve", lambda e: e.scalar_tensor_tensor(st_[:, nq * 512:(nq + 1) * 512], po[:], comb[:, tt, ex:ex + 1], st_[:, nq * 512:(nq + 1) * 512],
                                                                                 op0=ALU.mult, op1=ALU.add), reads=[po, comb, st_], writes=[st_])
                            S.dma("sp", macc_d[tt * P:(tt + 1) * P, :], st_[:], reads=[st_], writes=[macc_t[tt]])
                g2, b2, lnj = wgus[0], wgus[0], wgus[1]
                g2f = wgus[0][:].rearrange("p a b -> p (a b)").bitcast(F32)
                S.dma("sp", g2f[:, 0:D], ln2_g[l:l + 1, :].to_broadcast([P, D]), writes=[g2])
                S.dma("sp", g2f[:, D:2 * D], ln2_b[l:l + 1, :].to_broadcast([P, D]), writes=[b2])
                lnj_ap = wgus[1][:].rearrange("p a b -> p (a b)")[:, 0:D]
                S.dma("pool", wd[0:NE, 0, :], b_down[l, :, :], writes=[wd])
                for tt in range(NT):
                    a_, b_ = stg[0], stg[1]
                    S.dma("sp", a_[:], x1_d[tt * P:(tt + 1) * P, :], reads=[x1_t[tt]], writes=[a_])
                    S.dma("sp", b_[:], macc_d[tt * P:(tt + 1) * P, :], reads=[macc_t[tt]], writes=[b_])
                    S.op("dve", lambda e: e.scalar_tensor_tensor(a_[:], a_[:], float(ALPHA), b_[:], op0=ALU.mult, op1=ALU.add), reads=[a_, b_], writes=[a_])
                    S.op("act", lambda e: e.copy(cb16[:], comb[:, tt, :]), reads=[comb], writes=[cb16])
                    pc = ps_mm()
                    S.op("pe", lambda e: e.matmul(pc[0:NE, 0:P], lhsT=cb16[:, :], rhs=ident[:], start=True, stop=True), reads=[cb16, ident], writes=[pc])
                    S.op("act", lambda e: e.copy(combT[:], pc[0:NE, 0:P]), reads=[pc], writes=[combT])
                    for nq in range(4):
                        po = ps_acc()
                        S.op("pe", lambda e: e.matmul(po[:], lhsT=combT[:, :], rhs=wd[0:NE, 0, nq * 512:(nq + 1) * 512], start=True, stop=True),
                             reads=[combT, wd], writes=[po])
                        S.op("dve", lambda e: e.tensor_tensor(a_[:, nq * 512:(nq + 1) * 512], a_[:, nq * 512:(nq + 1) * 512], po[:], ALU.add),
                             reads=[po, a_], writes=[a_])
                    layer_norm_tile(a_, a_[:], g2, g2f[:, 0:D], b2, g2f[:, D:2 * D], {"st": lnst, "junk": lnj, "junk_ap": lnj_ap})
                    S.dma("sp", dst_d[tt * P:(tt + 1) * P, :], a_[:], reads=[a_], writes=[dst_t[tt]])
                S.barrier(moe_bufs)

        if not do_moe:
            with ExitStack() as ms:
                tmp = sb(ms, "cp", [P, D], F32)
                for tt in range(NT):
                    S.dma("sp", tmp[:], x1_d[tt * P:(tt + 1) * P, :], reads=[x1_t[tt]], writes=[tmp])
                    S.dma("sp", y_out[tt * P:(tt + 1) * P, :], tmp[:], reads=[tmp], writes=[y_t[tt]])
                S.barrier([tmp])
        S.drain_dmas("sp")
        print("instr counts", S.tot, "sems", S.nsem)
    return nc


def _col_perm():
    aq, ak, av, iq, ik, iw = 0, 1024, 1280, 1536, 2048, 2112
    cb, cc, ch, rq, rk, rv, rg = 2120, 2632, 3144, 3656, 4168, 4680, 5192
    idx = []
    for pg in range(2):
        for r in range(4):
            for g in (2 * pg, 2 * pg + 1):
                h = 4 * g + r
                idx += list(range(aq + h * 64, aq + h * 64 + 64))
    idx += list(range(ak, ak + 256))
    idx += list(range(iq, iq + 512))
    idx += list(range(ik, ik + 64)) * 2
    for j in range(4):
        idx += list(range(cb + j * 128, cb + (j + 1) * 128)) + list(range(cc + j * 128, cc + (j + 1) * 128)) \
            + list(range(ch + j * 128, ch + (j + 1) * 128))
    idx += list(range(rq, rq + 512)) + list(range(rk, rk + 512)) + list(range(rv, rv + 512)) + list(range(rg, rg + 512))
    idx += list(range(av, av + 256)) + list(range(iw, iw + 8))
    assert len(idx) == NCOL
    return np.asarray(idx)


def _consts():
    P = 128
    t = np.arange(P)
    trineg = np.where(t[None, :] <= t[:, None], 0.0, -1e30).astype(np.float32)
    triT = (t[None, :] >= t[:, None]).astype(np.float32)
    tri4 = np.tile(triT, (1, 4))
    pow2 = np.tile((2.0 ** -(np.arange(NBIS) + 1.0))[None, :], (P, 1)).astype(np.float32)
    half = 64
    inv_freq = (10000.0 ** (-np.linspace(0.0, 1.0, half, dtype=np.float32))).astype(np.float32)
    pos = np.arange(L, dtype=np.float32)
    ang = (pos[:, None] * inv_freq[None, :]).astype(np.float32).astype(np.float64)
    cos, sin = np.cos(ang), np.sin(ang)
    i = (np.arange(L) % 128).astype(np.float64)
    rot = np.zeros((4, L, 4, 64), np.float64)
    for h in range(4):
        lgm = np.log1p(-(2.0 ** (-5.0 - h)))
        qd = np.exp((i + 1.0) * lgm)[:, None]
        kd = (128.0 ** -0.5) * np.exp(-(i + 1.0) * lgm)[:, None]
        rot[0, :, h] = cos * qd; rot[1, :, h] = sin * qd
        rot[2, :, h] = cos * kd; rot[3, :, h] = sin * kd
    return {"c_ident": np.eye(P, dtype=np.float32), "c_trineg": trineg, "c_tri4": tri4.astype(np.float32),
            "c_pow2": pow2, "c_rot": rot.reshape(4, L, 256).astype(np.float32)}


_CACHE = {}


def _run(inputs, n_layers=DEPTH, do_moe=True, dbg=False, n_cores=2):
    key = (n_layers, do_moe, dbg)
    if key not in _CACHE:
        _CACHE[key] = build_program(n_layers, do_moe, dbg)
    nc = _CACHE[key]
    perm = _col_perm()
    f = lambda a: np.ascontiguousarray(np.asarray(a, dtype=np.float32))
    shared = {
        "w_in": f(np.asarray(inputs["w_in"])[:n_layers][:, :, perm]),
        "conv_w": f(inputs["conv_w"][:n_layers]), "w_out": f(inputs["w_out"][:n_layers]),
        "ln1_g": f(inputs["ln1_g"][:n_layers]), "ln1_b": f(inputs["ln1_b"][:n_layers]),
        "ln2_g": f(inputs["ln2_g"][:n_layers]), "ln2_b": f(inputs["ln2_b"][:n_layers]),
    }
    if do_moe:
        for k in ("router_w", "router_b", "w_gu", "b_gu", "w_down", "b_down"):
            shared[k] = f(inputs[k][:n_layers])
    shared.update(_consts())
    x = np.asarray(inputs["x"], dtype=np.float32)
    in_maps = [dict(shared, x=np.ascontiguousarray(x[b])) for b in range(n_cores)]
    res = run_bass_kernel_spmd(nc, in_maps, core_ids=list(range(n_cores)))
    return res


def kernel(**inputs):
    res = _run(inputs)
    return np.stack([res.results[b]["y"] for b in range(2)], axis=0).astype(np.float32)
```
